# Optimizing a Trainium2 kernel written in Bass

```python
import math
import jax
import jax.numpy as jnp
from jax import lax
import numpy as np

D_MODEL = 2048
BATCH = 8
SEQ = 2048
DEPTH = 2

HEAD_DIM = 64
N_HEADS = D_MODEL // HEAD_DIM
N_HEADS_SB = N_HEADS // 4
N_HEADS_DIL = (N_HEADS - N_HEADS_SB) // 2
N_HEADS_FOX = N_HEADS - N_HEADS_SB - N_HEADS_DIL
MIX_WIDTH = N_HEADS * HEAD_DIM
W_SB = N_HEADS_SB * HEAD_DIM
W_FOX = N_HEADS_FOX * HEAD_DIM
W_DIL = N_HEADS_DIL * HEAD_DIM
IN_SIZES = (W_SB, W_SB, W_SB, W_FOX, W_FOX, W_FOX, W_DIL, W_DIL, W_DIL, N_HEADS_FOX)
VALUE_SLOTS = (2, 5, 8)
IN_WIDTH = sum(IN_SIZES)

QUERY_BLOCK = 128
DILATED_PATTERNS = ((128, 1), (512, 4), (2048, 16))
ROPE_THETA = 10000.0
FORGET_BIAS_INIT = 4.0

N_EXPERTS = 64
TOP_K = 6
EXPERT_HIDDEN = 512
SHARED_HIDDEN = 512
ROUTED_SCALE = 2.5
MOE_BLOCK = 128

DEEPNORM_ALPHA = (2 * DEPTH) ** 0.25
DEEPNORM_BETA = (8 * DEPTH) ** -0.25
LN_EPS = 1e-5
RMS_EPS = 1e-6

kernel_name = 'hybrid_stickbreak_fox_dilated_moe_deepnorm_adaln'


def layer_norm(x, g, b):
    xf = x.astype(jnp.float32)
    mu = jnp.mean(xf, axis=-1, keepdims=True)
    var = jnp.mean(jnp.square(xf - mu), axis=-1, keepdims=True)
    return ((xf - mu) * lax.rsqrt(var + LN_EPS) * g + b).astype(x.dtype)


def rms_norm(x, g):
    xf = x.astype(jnp.float32)
    return (xf * lax.rsqrt(jnp.mean(jnp.square(xf), axis=-1, keepdims=True) + RMS_EPS) * g).astype(x.dtype)


def split_columns(proj):
    parts, off = [], 0
    for n in IN_SIZES:
        parts.append(proj[..., off:off + n])
        off += n
    return parts


def to_heads(t):
    b, s, w = t.shape
    return t.reshape(b, s, w // HEAD_DIM, HEAD_DIM).transpose(0, 2, 1, 3)


def rope(t, positions):
    half = HEAD_DIM // 2
    inv_freq = ROPE_THETA ** (-jnp.arange(half, dtype=jnp.float32) / half)
    ang = positions.astype(jnp.float32)[:, None, :, None] * inv_freq
    cos, sin = jnp.cos(ang), jnp.sin(ang)
    tf = t.astype(jnp.float32)
    t1, t2 = tf[..., :half], tf[..., half:]
    return jnp.concatenate([t1 * cos - t2 * sin, t1 * sin + t2 * cos], axis=-1).astype(t.dtype)


def sweep_query_blocks(block_fn, seq_len):
    n_blocks = seq_len // QUERY_BLOCK
    out = lax.map(block_fn, jnp.arange(n_blocks))
    out = jnp.moveaxis(out, 0, 2)
    return out.reshape(out.shape[:2] + (seq_len, out.shape[-1]))


def stick_breaking_attention(q, k, v):
    seq_len = q.shape[2]
    key_idx = jnp.arange(seq_len)

    def block(b):
        start = b * QUERY_BLOCK
        qb = lax.dynamic_slice_in_dim(q, start, QUERY_BLOCK, axis=2)
        z = jnp.einsum('bhqd,bhkd->bhqk', qb, k, preferred_element_type=jnp.float32) * HEAD_DIM ** -0.5
        q_idx = start + jnp.arange(QUERY_BLOCK)
        past = key_idx[None, :] < q_idx[:, None]
        log_keep = jnp.where(past, jax.nn.log_sigmoid(-z), 0.0)
        later = lax.cumsum(log_keep, axis=3, reverse=True) - log_keep
        w = jnp.where(past, jnp.exp(jax.nn.log_sigmoid(z) + later), 0.0)
        return jnp.einsum('bhqk,bhkd->bhqd', w.astype(v.dtype), v)

    return sweep_query_blocks(block, seq_len)


def forgetting_attention(q, k, v, log_f):
    seq_len = q.shape[2]
    key_idx = jnp.arange(seq_len)
    cum = jnp.cumsum(log_f, axis=-1)

    def block(b):
        start = b * QUERY_BLOCK
        qb = lax.dynamic_slice_in_dim(q, start, QUERY_BLOCK, axis=2)
        cum_q = lax.dynamic_slice_in_dim(cum, start, QUERY_BLOCK, axis=2)
        z = jnp.einsum('bhqd,bhkd->bhqk', qb, k, preferred_element_type=jnp.float32) * HEAD_DIM ** -0.5
        logits = z + (cum_q[..., :, None] - cum[..., None, :])
        q_idx = start + jnp.arange(QUERY_BLOCK)
        causal = key_idx[None, :] <= q_idx[:, None]
        p = jax.nn.softmax(jnp.where(causal, logits, -jnp.inf), axis=-1)
        return jnp.einsum('bhqk,bhkd->bhqd', p.astype(v.dtype), v)

    return sweep_query_blocks(block, seq_len)


def sliding_window_attention(q, k, v, window):
    length = q.shape[-2]
    blk = math.gcd(length, QUERY_BLOCK)
    n_blocks = length // blk
    span = window + blk
    pad = [(0, 0)] * (q.ndim - 2) + [(window, 0), (0, 0)]
    kp, vp = jnp.pad(k, pad), jnp.pad(v, pad)
    starts = jnp.arange(n_blocks) * blk
    slab = starts[:, None] + jnp.arange(span)[None, :]
    ks = jnp.take(kp, slab, axis=-2)
    vs = jnp.take(vp, slab, axis=-2)
    qb = q.reshape(q.shape[:-2] + (n_blocks, blk, q.shape[-1]))
    logits = jnp.einsum('...nqd,...nkd->...nqk', qb, ks, preferred_element_type=jnp.float32)
    dist = jnp.arange(blk)[:, None] + window - jnp.arange(span)[None, :]
    key_pos = starts[:, None] - window + jnp.arange(span)[None, :]
    valid = ((dist >= 0) & (dist <= window))[None] & (key_pos >= 0)[:, None, :]
    logits = jnp.where(valid, logits, -jnp.inf)
    m = jnp.max(logits, axis=-1, keepdims=True)
    p = jnp.exp(logits - m)
    denom = jnp.sum(p, axis=-1)
    o = jnp.einsum('...nqk,...nkd->...nqd', p.astype(v.dtype), vs, preferred_element_type=jnp.float32)
    o = o / denom[..., None]
    lse = m[..., 0] + jnp.log(denom)
    return o.reshape(q.shape).astype(v.dtype), lse.reshape(q.shape[:-1])


def to_residues(t, dil):
    b, h, s = t.shape[:3]
    return jnp.swapaxes(t.reshape((b, h, s // dil, dil) + t.shape[3:]), 2, 3)


def from_residues(t):
    t = jnp.swapaxes(t, 2, 3)
    return t.reshape(t.shape[:2] + (t.shape[2] * t.shape[3],) + t.shape[4:])


def dilated_attention(q, k, v):
    q = q * HEAD_DIM ** -0.5
    outs, lses = [], []
    for window, dil in DILATED_PATTERNS:
        o, lse = sliding_window_attention(to_residues(q, dil), to_residues(k, dil),
                                          to_residues(v, dil), window // dil)
        outs.append(from_residues(o))
        lses.append(from_residues(lse))
    wts = jax.nn.softmax(jnp.stack(lses), axis=0)
    o = jnp.sum(wts[..., None] * jnp.stack(outs).astype(jnp.float32), axis=0)
    return o.astype(v.dtype)


def token_mixer(h, positions, w_in, b_forget, head_norm_g, w_out):
    b, s, _ = h.shape
    proj = h @ w_in
    (q_sb, k_sb, v_sb, q_fx, k_fx, v_fx, q_dl, k_dl, v_dl, f_logit) = split_columns(proj)
    o_sb = stick_breaking_attention(to_heads(q_sb), to_heads(k_sb), to_heads(v_sb))
    log_f = jax.nn.log_sigmoid((f_logit + b_forget).astype(jnp.float32)).transpose(0, 2, 1)
    o_fx = forgetting_attention(to_heads(q_fx), to_heads(k_fx), to_heads(v_fx), log_f)
    o_dl = dilated_attention(rope(to_heads(q_dl), positions), rope(to_heads(k_dl), positions),
                             to_heads(v_dl))
    o = jnp.concatenate([o_sb, o_fx, o_dl], axis=1)
    o = rms_norm(o, head_norm_g.reshape(N_HEADS, 1, HEAD_DIM))
    o = o.transpose(0, 2, 1, 3).reshape(b, s, MIX_WIDTH)
    return o @ w_out


def swiglu(x, w_gate, w_up, w_down):
    return (jax.nn.silu(x @ w_gate) * (x @ w_up)) @ w_down


def grouped_experts(hf, expert_idx, gates, w_gate, w_up, w_down):
    n_tok = hf.shape[0]
    n_assign = n_tok * TOP_K
    flat_e = expert_idx.reshape(-1)
    flat_tok = jnp.arange(n_assign, dtype=jnp.int32) // TOP_K
    flat_w = gates.reshape(-1)
    order = jnp.argsort(flat_e, stable=True)
    e_sorted = flat_e[order]
    counts = jnp.bincount(flat_e, length=N_EXPERTS)
    padded = (counts + MOE_BLOCK - 1) // MOE_BLOCK * MOE_BLOCK
    start = jnp.cumsum(counts) - counts
    pad_end = jnp.cumsum(padded)
    pad_start = pad_end - padded
    dest = pad_start[e_sorted] + jnp.arange(n_assign, dtype=jnp.int32) - start[e_sorted]
    n_blocks = -(-(n_assign + N_EXPERTS * (MOE_BLOCK - 1)) // MOE_BLOCK)
    rows = n_blocks * MOE_BLOCK
    row_tok = jnp.zeros((rows,), jnp.int32).at[dest].set(flat_tok[order])
    row_w = jnp.zeros((rows,), flat_w.dtype).at[dest].set(flat_w[order])
    block_e = jnp.searchsorted(pad_end, jnp.arange(n_blocks) * MOE_BLOCK, side='right')
    block_e = jnp.minimum(block_e, N_EXPERTS - 1)

    def step(acc, blk):
        tok, wt, e = blk
        y = swiglu(hf[tok], w_gate[e], w_up[e], w_down[e]) * wt[:, None]
        return acc.at[tok].add(y), None

    acc, _ = lax.scan(step, jnp.zeros_like(hf),
                      (row_tok.reshape(n_blocks, MOE_BLOCK), row_w.reshape(n_blocks, MOE_BLOCK), block_e))
    return acc


def moe_ffn(h, w_router, router_bias, w_gate, w_up, w_down, ws_gate, ws_up, ws_down):
    b, s, d = h.shape
    hf = h.reshape(b * s, d)
    scores = jax.nn.sigmoid(hf.astype(jnp.float32) @ w_router.astype(jnp.float32))
    _, idx = lax.top_k(scores + router_bias.astype(jnp.float32), TOP_K)
    sel = jnp.take_along_axis(scores, idx, axis=-1)
    gates = (sel / jnp.sum(sel, axis=-1, keepdims=True) * ROUTED_SCALE).astype(h.dtype)
    routed = grouped_experts(hf, idx, gates, w_gate, w_up, w_down)
    shared = swiglu(hf, ws_gate, ws_up, ws_down)
    return (routed + shared).reshape(b, s, d)


def setup_inputs(seed: int = 0) -> dict:
    key = jax.random.key(seed)
    ks = jax.random.split(key, 22)
    d, L, e, fh, sh = D_MODEL, DEPTH, N_EXPERTS, EXPERT_HIDDEN, SHARED_HIDDEN

    def dense(k, shape, fan_in, gain=1.0):
        return jax.random.normal(k, shape, jnp.float32) * (gain * fan_in ** -0.5)

    def near_one(k, shape):
        return 1.0 + 0.02 * jax.random.normal(k, shape, jnp.float32)

    def small(k, shape, scale=0.02):
        return scale * jax.random.normal(k, shape, jnp.float32)

    col_gain = jnp.concatenate([jnp.full((n,), DEEPNORM_BETA if i in VALUE_SLOTS else 1.0, jnp.float32)
                                for i, n in enumerate(IN_SIZES)])
    offsets = jax.random.randint(ks[2], (BATCH, 1), 0, SEQ, dtype=jnp.int32)
    return {
        'x': jax.random.normal(ks[0], (BATCH, SEQ, d), jnp.float32),
        'c': jax.random.normal(ks[1], (BATCH, d), jnp.float32),
        'positions': offsets + jnp.arange(SEQ, dtype=jnp.int32)[None, :],
        'w_ada': dense(ks[3], (L, d, 6 * d), d),
        'b_ada': small(ks[4], (L, 6 * d)),
        'w_in': dense(ks[5], (L, d, IN_WIDTH), d) * col_gain,
        'b_forget': FORGET_BIAS_INIT + small(ks[6], (L, N_HEADS_FOX), 0.1),
        'head_norm_g': near_one(ks[7], (L, MIX_WIDTH)),
        'w_out': dense(ks[8], (L, MIX_WIDTH, d), MIX_WIDTH, DEEPNORM_BETA),
        'ln1_g': near_one(ks[9], (L, d)),
        'ln1_b': small(ks[10], (L, d)),
        'w_router': dense(ks[11], (L, d, e), d),
        'router_bias': small(ks[12], (L, e), 0.01),
        'w_exp_gate': dense(ks[13], (L, e, d, fh), d, DEEPNORM_BETA),
        'w_exp_up': dense(ks[14], (L, e, d, fh), d, DEEPNORM_BETA),
        'w_exp_down': dense(ks[15], (L, e, fh, d), fh, DEEPNORM_BETA),
        'w_sh_gate': dense(ks[16], (L, d, sh), d, DEEPNORM_BETA),
        'w_sh_up': dense(ks[17], (L, d, sh), d, DEEPNORM_BETA),
        'w_sh_down': dense(ks[18], (L, sh, d), sh, DEEPNORM_BETA),
        'ln2_g': near_one(ks[19], (L, d)),
        'ln2_b': small(ks[20], (L, d)),
    }


def reference(x, c, positions, w_ada, b_ada, w_in, b_forget, head_norm_g, w_out, ln1_g, ln1_b,
              w_router, router_bias, w_exp_gate, w_exp_up, w_exp_down, w_sh_gate, w_sh_up, w_sh_down,
              ln2_g, ln2_b):
    c_act = jax.nn.silu(c)
    for l in range(DEPTH):
        mod = (c_act @ w_ada[l] + b_ada[l])[:, None, :]
        shift1, scale1, gate1, shift2, scale2, gate2 = jnp.split(mod, 6, axis=-1)
        h = x * (1.0 + scale1) + shift1
        mix = token_mixer(h, positions, w_in[l], b_forget[l], head_norm_g[l], w_out[l])
        x = layer_norm(DEEPNORM_ALPHA * x + gate1 * mix, ln1_g[l], ln1_b[l])
        h = x * (1.0 + scale2) + shift2
        ffn = moe_ffn(h, w_router[l], router_bias[l], w_exp_gate[l], w_exp_up[l], w_exp_down[l],
                      w_sh_gate[l], w_sh_up[l], w_sh_down[l])
        x = layer_norm(DEEPNORM_ALPHA * x + gate2 * ffn, ln2_g[l], ln2_b[l])
    return x
```

```python
import math
from contextlib import ExitStack
import numpy as np
import concourse.bass as bass
import concourse.mybir as mybir
from concourse.bass_utils import run_bass_kernel_spmd

F32 = mybir.dt.float32
BF16 = mybir.dt.bfloat16
I32 = mybir.dt.int32
AF = mybir.ActivationFunctionType
ALU = mybir.AluOpType
AX = mybir.AxisListType

D = 2048
SEQ = 2048
NT = 16
KC = 16
DEPTH = 2
ALPHA = float((2 * DEPTH) ** 0.25)
NE = 64
EH = 512
INW = 6156
PI = math.pi
N_CORES = 8
DEBUG = {}


class Sched:
    def __init__(self, nc):
        self.nc = nc
        self.E = {'pe': nc.tensor, 'act': nc.scalar, 'dve': nc.vector, 'pool': nc.gpsimd, 'sp': nc.sync}
        self.sem = {}
        self.cnt = {}
        for k in self.E:
            self.sem[k] = nc.semaphore('s_' + k).__enter__()
            self.cnt[k] = 0
        self.seen = {k: {} for k in self.E}
        self.res = {}
        self.dmap = {}
        self.dpool = []
        self.dfree = []

    def _phys(self, name):
        if name not in self.dmap:
            if self.dfree:
                p = self.dfree.pop()
            else:
                p = 'd%d' % len(self.dpool)
                self.dpool.append(p)
                self.sem[p] = self.nc.semaphore('s_' + p).__enter__()
                self.cnt[p] = 0
            self.dmap[name] = p
        return self.dmap[name]

    def op(self, eng, fn, reads=(), writes=(), dsem=None):
        deps = {}

        def add(tag):
            if tag is not None and deps.get(tag[0], 0) < tag[1]:
                deps[tag[0]] = tag[1]
        for r in reads:
            rec = self.res.get(r)
            if rec:
                add(rec['w'])
        for w in writes:
            rec = self.res.get(w)
            if rec:
                add(rec['w'])
                for k, v in rec['r'].items():
                    add((k, v))
        seen = self.seen[eng]
        for k, v in deps.items():
            if k == eng and eng == 'pe' and dsem is None:
                continue
            if seen.get(k, 0) >= v:
                continue
            self.E[eng].wait_ge(self.sem[k], v)
            seen[k] = v
        ins = fn(self.E[eng])
        if dsem is None:
            self.cnt[eng] += 1
            ins.then_inc(self.sem[eng], 1)
            tag = (eng, self.cnt[eng])
        else:
            p = self._phys(dsem)
            self.cnt[p] += 16
            ins.then_inc(self.sem[p], 16)
            tag = (p, self.cnt[p])
        for r in reads:
            rec = self.res.setdefault(r, {'w': None, 'r': {}})
            if rec['r'].get(tag[0], 0) < tag[1]:
                rec['r'][tag[0]] = tag[1]
        for w in writes:
            self.res[w] = {'w': tag, 'r': {}}
        return ins

    def barrier(self):
        snap = dict(self.cnt)
        for eng in self.E:
            for k, v in snap.items():
                if v > 0 and k != eng and self.seen[eng].get(k, 0) < v:
                    self.E[eng].wait_ge(self.sem[k], v)
                    self.seen[eng][k] = v
        self.res = {}
        self.dfree = list(self.dpool)
        self.dmap = {}

    def finish(self, eng='sp'):
        for k, v in self.cnt.items():
            if v > 0 and k != eng and self.seen[eng].get(k, 0) < v:
                self.E[eng].wait_ge(self.sem[k], v)
                self.seen[eng][k] = v


class Ctx:
    pass


_UID = [0]


def _alloc(es, nc, name, shape, dt):
    _UID[0] += 1
    return es.enter_context(nc.sbuf_tensor('%s_%d' % (name, _UID[0]), list(shape), dt))


def _palloc(es, nc, name, shape, dt):
    _UID[0] += 1
    return es.enter_context(nc.psum_tensor('%s_%d' % (name, _UID[0]), list(shape), dt))


def mm(S, out, lhsT, rhs, start, stop, reads, writes):
    return S.op('pe', lambda e: e.matmul(out, lhsT, rhs, start=start, stop=stop), reads, writes)


def tr(S, out, in_, ident, reads, writes):
    return S.op('pe', lambda e: e.transpose(out, in_, ident), reads, writes)


def act(S, out, in_, func, reads, writes, bias=None, scale=None):
    kw = {}
    if bias is not None:
        kw['bias'] = bias
    if scale is not None:
        kw['scale'] = scale
    return S.op('act', lambda e: e.activation(out=out, in_=in_, func=func, **kw), reads, writes)


def tt(S, out, in0, in1, op, reads, writes, eng='dve'):
    return S.op(eng, lambda e: e.tensor_tensor(out=out, in0=in0, in1=in1, op=op), reads, writes)


def ts(S, out, in0, s1, s2, op0, op1, reads, writes, eng='dve'):
    if op1 is None:
        return S.op(eng, lambda e: e.tensor_scalar(out=out, in0=in0, scalar1=s1, scalar2=None, op0=op0), reads, writes)
    return S.op(eng, lambda e: e.tensor_scalar(out=out, in0=in0, scalar1=s1, scalar2=s2, op0=op0, op1=op1), reads, writes)


def stt(S, out, in0, scalar, in1, op0, op1, reads, writes):
    return S.op('dve', lambda e: e.scalar_tensor_tensor(out=out, in0=in0, scalar=scalar, in1=in1, op0=op0, op1=op1),
                reads, writes)


def cp(S, eng, out, in_, reads, writes):
    if eng == 'act':
        return S.op('act', lambda e: e.activation(out=out, in_=in_, func=AF.Copy), reads, writes)
    return S.op(eng, lambda e: e.tensor_copy(out=out, in_=in_), reads, writes)


def dma(S, q, out, in_, reads, writes, dsem, **kw):
    if q == 'pool' and 'max_dma_last_dim' not in kw:
        kw['max_dma_last_dim'] = 4096
    return S.op(q, lambda e: e.dma_start(out=out, in_=in_, **kw), reads, writes, dsem=dsem)


def layer_norm_tile(C, S, es_t, y, yk, outt, outk, g, gk, b, bk, tag):
    t = es_t
    st, mv, lnv, rstd = t['st'], t['mv'], t['lnv'], t['rstd']
    for i in range(4):
        S.op('dve', lambda e: e.bn_stats(out=st[:, i * 6:(i + 1) * 6], in_=y[:, i * 512:(i + 1) * 512]), [yk], [(tag, 'st', i)])
    S.op('dve', lambda e: e.bn_aggr(out=mv[:, :], in_=st[:, :]), [(tag, 'st', i) for i in range(4)], [(tag, 'mv')])
    act(S, lnv[:, :], mv[:, 1:2], AF.Ln, [(tag, 'mv')], [(tag, 'lnv')], bias=C.eps5[:, 0:1])
    act(S, rstd[:, :], lnv[:, :], AF.Exp, [(tag, 'lnv')], [(tag, 'rstd')], scale=-0.5)
    ts(S, y[:, :], y[:, :], mv[:, 0:1], rstd[:, 0:1], ALU.subtract, ALU.mult, [yk, (tag, 'mv'), (tag, 'rstd')], [yk])
    tt(S, y[:, :], y[:, :], g[:, :], ALU.mult, [yk, gk], [yk])
    tt(S, outt[:, :], y[:, :], b[:, :], ALU.add, [yk, bk], [outk])


def phase_adaln(C, S, l):
    nc = C.nc
    with ExitStack() as es:
        c16 = _alloc(es, nc, 'c16', [16, 128], F32)
        cact = _alloc(es, nc, 'cact', [128, 16], F32)
        crep = _alloc(es, nc, 'crep', [128, 16, 128], F32)
        wsl = [_alloc(es, nc, 'wada%d' % i, [128, 16, 512], F32) for i in range(2)]
        brow = [_alloc(es, nc, 'brow%d' % i, [1, 512], F32) for i in range(2)]
        stg = [_alloc(es, nc, 'mstg%d' % i, [128, 512], F32) for i in range(2)]
        pT = _palloc(es, nc, 'pT', [128, 16], F32)
        pm = [_palloc(es, nc, 'pmod%d' % i, [128, 512], F32) for i in range(2)]
        dma(S, 'sp', c16[:, :], C.c[:, :], [], ['c16'], 'c16')
        tr(S, pT[:, 0:16], c16[0:16, :], C.ident_f[0:16, 0:16], ['c16'], ['pT'])
        act(S, cact[:, :], pT[:, :], AF.Silu, ['pT'], ['cact'])
        for k in range(KC):
            act(S, crep[:, k, :], C.ones_f[:, :], AF.Identity, ['cact'], [('crep', k)], scale=cact[:, k:k + 1])
        wv = C.w_ada[l].rearrange("(k p) n -> p k n", p=128)
        for j in range(24):
            s = j % 2
            dma(S, 'sp', wsl[s][:, :, :], wv[:, :, j * 512:(j + 1) * 512], [], [('wada', s)], 'wada%d' % s)
            dma(S, 'sp', brow[s][0:1, :], C.b_ada[l:l + 1, j * 512:(j + 1) * 512], [], [('brow', s)], 'brow%d' % s)
            for k in range(KC):
                mm(S, pm[s][:, :], crep[:, k, :], wsl[s][:, k, :], k == 0, False,
                   [('crep', k), ('wada', s)], [('pmod', s)])
            mm(S, pm[s][:, :], C.ones_f[0:1, :], brow[s][0:1, :], False, True, [('brow', s)], [('pmod', s)])
            cp(S, 'act' if j % 2 else 'dve', stg[s][:, :], pm[s][:, :], [('pmod', s)], [('mstg', s)])
            dma(S, 'sp', C.modb[:, j * 512:(j + 1) * 512], stg[s][:, :], [('mstg', s)], [('modb', j // 4)], 'mst%d' % s)
    S.barrier()


def phase_m1(C, S, l, xsrc, hT):
    nc = C.nc
    with ExitStack() as es:
        sc1 = _alloc(es, nc, 'sc1', [128, D], F32)
        sh1 = _alloc(es, nc, 'sh1', [128, D], F32)
        xs = [_alloc(es, nc, 'm1x%d' % i, [128, D], F32) for i in range(2)]
        hb = [_alloc(es, nc, 'm1h%d' % i, [128, D], BF16) for i in range(2)]
        tp = [_palloc(es, nc, 'm1tp%d' % i, [128, 1024], BF16) for i in range(2)]
        dma(S, 'sp', sc1[:, :], C.modb[:, 2048:4096], [], ['sc1'], 'sc1')
        dma(S, 'sp', sh1[:, :], C.modb[:, 0:2048], [], ['sh1'], 'sh1')
        ts(S, sc1[:, :], sc1[:, :], 1.0, None, ALU.add, None, ['sc1'], ['sc1'])
        for t in range(NT):
            s = t % 2
            dma(S, 'sp', xs[s][:, :], xsrc[t * 128:(t + 1) * 128, :], [('xr', t)], [('m1x', s)], 'm1x%d' % s)
            tt(S, xs[s][:, :], xs[s][:, :], sc1[:, :], ALU.mult, [('m1x', s), 'sc1'], [('m1x', s)])
            tt(S, hb[s][:, :], xs[s][:, :], sh1[:, :], ALU.add, [('m1x', s), 'sh1'], [('m1h', s)])
            for kk in range(2):
                for k8 in range(8):
                    k = kk * 8 + k8
                    tr(S, tp[kk][:, k8 * 128:(k8 + 1) * 128], hb[s][:, k * 128:(k + 1) * 128], C.ident_b[:, :],
                       [('m1h', s)], [('m1tp', kk)])
                cp(S, 'act' if kk == 0 else 'dve', hT[:, kk * 8:(kk + 1) * 8, t * 128:(t + 1) * 128],
                   tp[kk][:, :].rearrange("p (a b) -> p a b", a=8), [('m1tp', kk)], [('hT', t)])
    S.barrier()


def phase_m2(C, S, l, hT):
    nc = C.nc
    hT_all = [('hT', t) for t in range(NT)]
    with ExitStack() as es:
        NW = 3
        wsl = [_alloc(es, nc, 'm2w%d' % i, [128, 16, 512], BF16) for i in range(NW)]
        stT = [_alloc(es, nc, 'stT%d' % i, [128, SEQ], BF16) for i in range(2)]
        stV = [_alloc(es, nc, 'stV%d' % i, [128, 512], BF16) for i in range(2)]
        stD = [_alloc(es, nc, 'stD%d' % i, [128, 3, SEQ], BF16) for i in range(2)]
        cos3 = _alloc(es, nc, 'cos3', [128, 16, 6, 32], F32)
        sin3 = _alloc(es, nc, 'sin3', [128, 16, 6, 32], F32)
        ppos = _palloc(es, nc, 'm2pp', [128, 16], F32)
        with ExitStack() as er:
            posi = _alloc(er, nc, 'posi', [16, 128], I32)
            posf = _alloc(er, nc, 'posf', [16, 128], F32)
            posT = _alloc(er, nc, 'posT', [128, 16], F32)
            invf = _alloc(er, nc, 'invf', [128, 32], F32)
            ang = _alloc(er, nc, 'ang', [128, 512], F32)
            kf = _alloc(er, nc, 'kf', [128, 512], F32)
            ki = _alloc(er, nc, 'ki', [128, 512], I32)
            rr = _alloc(er, nc, 'rr', [128, 512], F32)
            wr_ = _alloc(er, nc, 'wrp', [128, 512], F32)
            sinv = _alloc(er, nc, 'sinv', [128, 512], F32)
            cosv = _alloc(er, nc, 'cosv', [128, 512], F32)
            dma(S, 'sp', posi[:, :], C.pos[:, :], [], ['posi'], 'posi')
            dma(S, 'sp', invf[:, :], C.cst_invf[:, :], [], ['invf'], 'invf')
            cp(S, 'dve', posf[:, :], posi[:, :], ['posi'], ['posf'])
            tr(S, ppos[:, 0:16], posf[0:16, :], C.ident_f[0:16, 0:16], ['posf'], ['ppos'])
            cp(S, 'dve', posT[:, :], ppos[:, :], ['ppos'], ['posT'])
            for t in range(NT):
                ts(S, ang[:, t * 32:(t + 1) * 32], invf[:, :], posT[:, t:t + 1], None, ALU.mult, None,
                   ['invf', 'posT'], ['ang'])
            ts(S, kf[:, :], ang[:, :], 1.0 / (2 * PI), None, ALU.mult, None, ['ang'], ['kf'])
            cp(S, 'dve', ki[:, :], kf[:, :], ['kf'], ['ki'])
            cp(S, 'dve', kf[:, :], ki[:, :], ['ki'], ['kf'])
            stt(S, rr[:, :], kf[:, :], -2 * PI, ang[:, :], ALU.mult, ALU.add, ['kf', 'ang'], ['rr'])
            for (shift, dst, dk) in ((0.0, sinv, 'sinv'), (PI / 2, cosv, 'cosv')):
                if shift != 0.0:
                    ts(S, rr[:, :], rr[:, :], shift, None, ALU.add, None, ['rr'], ['rr'])
                for rep in range(2):
                    ts(S, wr_[:, :], rr[:, :], PI, 2 * PI, ALU.is_gt, ALU.mult, ['rr'], ['wrp'])
                    tt(S, rr[:, :], rr[:, :], wr_[:, :], ALU.subtract, ['rr', 'wrp'], ['rr'])
                    ts(S, wr_[:, :], rr[:, :], -PI, 2 * PI, ALU.is_lt, ALU.mult, ['rr'], ['wrp'])
                    tt(S, rr[:, :], rr[:, :], wr_[:, :], ALU.add, ['rr', 'wrp'], ['rr'])
                act(S, dst[:, :], rr[:, :], AF.Sin, ['rr'], [dk])
            for h in range(6):
                cp(S, 'dve', cos3[:, :, h, :], cosv[:, :].rearrange("p (t j) -> p t j", t=16), ['cosv'], ['cos3'])
                cp(S, 'dve', sin3[:, :, h, :], sinv[:, :].rearrange("p (t j) -> p t j", t=16), ['sinv'], ['sin3'])
            S.barrier()
        ra = [_alloc(es, nc, 'ra%d' % i, [128, 6, 32], F32) for i in range(4)]
        rb = [_alloc(es, nc, 'rb%d' % i, [128, 384], BF16) for i in range(2)]
        nbf = _alloc(es, nc, 'nbf', [12, 1], F32)
        lfe = _alloc(es, nc, 'lfe', [12, SEQ], F32)
        zer = _alloc(es, nc, 'zer', [12, SEQ], F32)
        cn = _alloc(es, nc, 'cn', [12, SEQ], F32)
        pj = [_palloc(es, nc, 'm2pj%d' % i, [128, 512], F32) for i in range(4)]
        ptr = [_palloc(es, nc, 'm2pt%d' % i, [128, 512], BF16) for i in range(2)]

        wv = C.w_in[l].rearrange("(k p) n -> p k n", p=128)
        groups = [('f', 6144, 12, None)]
        for u0 in range(4):
            pass
        groups += [('fm', 0, 512, [(u, 0) for u in range(4)]), ('fm', 512, 512, [(u, 1) for u in range(4)])]
        groups += [('tv', 1024, 512, 0)]
        groups += [('fm', 1536, 384, [(4 + i, 0) for i in range(3)]), ('fm', 1920, 384, [(7 + i, 0) for i in range(3)])]
        groups += [('fm', 2304, 384, [(4 + i, 1) for i in range(3)]), ('fm', 2688, 384, [(7 + i, 1) for i in range(3)])]
        groups += [('tv', 3072, 384, 512), ('tv', 3456, 384, 896)]
        groups += [('tr', 3840, 384, [(10 + i, 0) for i in range(3)]), ('tr', 4224, 384, [(13 + i, 0) for i in range(3)])]
        groups += [('tr', 4608, 384, [(10 + i, 1) for i in range(3)]), ('tr', 4992, 384, [(13 + i, 1) for i in range(3)])]
        groups += [('tv', 5376, 384, 1280), ('tv', 5760, 384, 1664)]
        nst = 0
        npj = 0
        ntd = 0
        for gi, (kind, c0, W, info) in enumerate(groups):
            ws = gi % NW
            dma(S, 'pool', wsl[ws][:, :, 0:W], wv[:, :, c0:c0 + W], [], [('m2w', ws)], 'm2w%d' % ws)
            wk = ('m2w', ws)
            if kind == 'fm':
                for ci, (u, qk) in enumerate(info):
                    s = nst % 2
                    nst += 1
                    for tb in range(4):
                        p = npj % 4
                        npj += 1
                        for k in range(KC):
                            mm(S, pj[p][:, :], wsl[ws][:, k, ci * 128:(ci + 1) * 128], hT[:, k, tb * 512:(tb + 1) * 512],
                               k == 0, k == KC - 1, [wk] + hT_all[tb * 4:tb * 4 + 4], [('pj', p)])
                        cp(S, 'act' if tb % 2 else 'dve', stT[s][:, tb * 512:(tb + 1) * 512], pj[p][:, :],
                           [('pj', p)], [('stT', s)])
                    dma(S, 'sp', C.qkT[u * 2 + qk], stT[s][:, :], [('stT', s)], [('qkT', u, qk)], 'stT%d' % s)
            elif kind == 'tv':
                for t in range(NT):
                    s = nst % 2
                    nst += 1
                    p = npj % 4
                    npj += 1
                    for k in range(KC):
                        mm(S, pj[p][:, 0:W], hT[:, k, t * 128:(t + 1) * 128], wsl[ws][:, k, 0:W],
                           k == 0, k == KC - 1, [wk, ('hT', t)], [('pj', p)])
                    cp(S, 'act' if t % 2 else 'dve', stV[s][:, 0:W], pj[p][:, 0:W], [('pj', p)], [('stV', s)])
                    dma(S, 'sp', C.vS[t * 128:(t + 1) * 128, info:info + W], stV[s][:, 0:W], [('stV', s)],
                        [('vS', info, t)], 'stV%d' % s)
            elif kind == 'tr':
                sd = ntd % 2
                ntd += 1
                for t in range(NT):
                    p = npj % 4
                    npj += 1
                    for k in range(KC):
                        mm(S, pj[p][:, 0:W], hT[:, k, t * 128:(t + 1) * 128], wsl[ws][:, k, 0:W],
                           k == 0, k == KC - 1, [wk, ('hT', t)], [('pj', p)])
                    pv = pj[p][:, 0:384].rearrange("p (h two j) -> p h two j", h=6, two=2)
                    t1 = pv[:, :, 0, :]
                    t2 = pv[:, :, 1, :]
                    s = t % 2
                    rbv = rb[s][:, :].rearrange("p (h two j) -> p h two j", h=6, two=2)
                    tt(S, ra[0][:, :, :], t1, cos3[:, t, :, :], ALU.mult, [('pj', p), 'cos3'], ['ra0'])
                    tt(S, ra[1][:, :, :], t2, sin3[:, t, :, :], ALU.mult, [('pj', p), 'sin3'], ['ra1'])
                    tt(S, rbv[:, :, 0, :], ra[0][:, :, :], ra[1][:, :, :], ALU.subtract, ['ra0', 'ra1'], [('rb', s, 0)])
                    tt(S, ra[2][:, :, :], t1, sin3[:, t, :, :], ALU.mult, [('pj', p), 'sin3'], ['ra2'])
                    tt(S, ra[3][:, :, :], t2, cos3[:, t, :, :], ALU.mult, [('pj', p), 'cos3'], ['ra3'])
                    tt(S, rbv[:, :, 1, :], ra[2][:, :, :], ra[3][:, :, :], ALU.add, ['ra2', 'ra3'], [('rb', s, 1)])
                    for c in range(3):
                        tr(S, ptr[s][:, c * 128:(c + 1) * 128], rb[s][:, c * 128:(c + 1) * 128], C.ident_b[:, :],
                           [('rb', s, 0), ('rb', s, 1)], [('ptr', s)])
                    cp(S, 'act', stD[sd][:, :, t * 128:(t + 1) * 128],
                       ptr[s][:, 0:384].rearrange("p (c j) -> p c j", c=3), [('ptr', s)], [('stD', sd)])
                for c, (u, qk) in enumerate(info):
                    dma(S, 'sp', C.qkT[u * 2 + qk], stD[sd][:, c, :], [('stD', sd)], [('qkT', u, qk)],
                        'stD%d_%d' % (sd, c))
            else:
                dma(S, 'sp', nbf[:, :], C.b_forget[l:l + 1, :].rearrange("o h -> h o"), [], ['nbf'], 'nbf',
                    allow_slow_non_contiguous=True)
                ts(S, nbf[:, :], nbf[:, :], -1.0, None, ALU.mult, None, ['nbf'], ['nbf'])
                S.op('dve', lambda e: e.memset(zer[:, :], 0.0), [], ['zer'])
                for tb in range(4):
                    p = npj % 4
                    npj += 1
                    for k in range(KC):
                        mm(S, pj[p][0:12, :], wsl[ws][:, k, 0:12], hT[:, k, tb * 512:(tb + 1) * 512],
                           k == 0, k == KC - 1, [wk] + hT_all[tb * 4:tb * 4 + 4], [('pj', p)])
                    act(S, lfe[:, tb * 512:(tb + 1) * 512], pj[p][0:12, :], AF.Exp, [('pj', p), 'nbf'], [('lfe', tb)],
                        bias=nbf[:, 0:1], scale=-1.0)
                    act(S, lfe[:, tb * 512:(tb + 1) * 512], lfe[:, tb * 512:(tb + 1) * 512], AF.Ln,
                        [('lfe', tb)], [('lfe', tb)], bias=C.one1[0:12, 0:1])
                S.op('dve', lambda e: e.tensor_tensor_scan(out=cn[:, :], data0=lfe[:, :], data1=zer[:, :], initial=0.0,
                                                           op0=ALU.add, op1=ALU.add),
                     [('lfe', tb) for tb in range(4)] + ['zer'], ['cn'])
                dma(S, 'sp', C.cneg[:, :], cn[:, :], ['cn'], ['cneg'], 'cn')
    S.barrier()


def phase_m3(C, S, l, OT):
    nc = C.nc
    with ExitStack() as es:
        msb = _alloc(es, nc, 'msb', [128, 896], BF16)
        mfx = _alloc(es, nc, 'mfx', [128, 896], BF16)
        mdl = _alloc(es, nc, 'mdl', [128, 2432], BF16)
        triu = _alloc(es, nc, 'triu', [128, 128], F32)
        g16 = _alloc(es, nc, 'g16', [16, 128], F32)
        gT = _alloc(es, nc, 'gT', [128, 16], F32)
        QT = [_alloc(es, nc, 'QT%d' % i, [128, SEQ], BF16) for i in range(2)]
        KT = [_alloc(es, nc, 'KT%d' % i, [128, SEQ], BF16) for i in range(2)]
        VV = [_alloc(es, nc, 'VV%d' % i, [128, 16, 128], BF16) for i in range(2)]
        cnq = [_alloc(es, nc, 'cnq%d' % i, [128, SEQ], F32) for i in range(2)]
        cnk = [_alloc(es, nc, 'cnk%d' % i, [128, 16], F32) for i in range(2)]
        NB = 2
        eT = [_alloc(es, nc, 'eT%d' % i, [128, 512], F32) for i in range(NB)]
        lT = [_alloc(es, nc, 'lT%d' % i, [128, 512], F32) for i in range(NB)]
        t1T = [_alloc(es, nc, 't1T%d' % i, [128, 512], F32) for i in range(NB)]
        t2T = [_alloc(es, nc, 't2T%d' % i, [128, 512], F32) for i in range(NB)]
        lmT = [_alloc(es, nc, 'lmT%d' % i, [128, 512], F32) for i in range(NB)]
        RT = [_alloc(es, nc, 'RT%d' % i, [128, 512], F32) for i in range(2)]
        wT = [_alloc(es, nc, 'wT%d' % i, [128, 512], BF16) for i in range(3)]
        wmT = [_alloc(es, nc, 'wmT%d' % i, [128, 512], BF16) for i in range(3)]
        o32 = [_alloc(es, nc, 'o32_%d' % i, [128, 512], F32) for i in range(2)]
        rsm = [_alloc(es, nc, 'rsm%d' % i, [128, 512], F32) for i in range(2)]
        sq = [_alloc(es, nc, 'sq%d' % i, [128, 512], BF16) for i in range(2)]
        lnv = [_alloc(es, nc, 'lnv%d' % i, [128, 512], F32) for i in range(2)]
        rstd = [_alloc(es, nc, 'rstd%d' % i, [128, 512], F32) for i in range(2)]
        zp = [_palloc(es, nc, 'zp%d' % i, [128, 512], F32) for i in range(2)]
        cpz = _palloc(es, nc, 'cpz', [128, 512], F32)
        opz = [_palloc(es, nc, 'opz%d' % i, [128, 512], F32) for i in range(2)]
        spz = [_palloc(es, nc, 'spz%d' % i, [128, 512], F32) for i in range(2)]
        mpz = _palloc(es, nc, 'mpz', [128, 512], F32)

        dma(S, 'pool', msb[:, :], C.cst_msb[:, :], [], ['msb'], 'msb')
        dma(S, 'pool', mfx[:, :], C.cst_mfx[:, :], [], ['mfx'], 'mfx')
        dma(S, 'pool', mdl[:, :], C.cst_mdl[:, :], [], ['mdl'], 'mdl')
        dma(S, 'sp', triu[:, :], C.cst_triu[:, :], [], ['triu'], 'triu')
        dma(S, 'sp', g16[:, :], C.hng[l], [], ['g16'], 'g16')
        tr(S, mpz[:, 0:16], g16[0:16, :], C.ident_f[0:16, 0:16], ['g16'], ['mpz'])
        cp(S, 'dve', gT[:, :], mpz[:, 0:16], ['mpz'], ['gT'])

        ti = 0
        fi = 0
        nfx = 0
        for u in range(16):
            kind = 'sb' if u < 4 else ('fx' if u < 10 else 'dl')
            us = u % 2
            dma(S, 'sp', QT[us][:, :], C.qkT[u * 2 + 0], [('qkT', u, 0)], [('QT', us)], 'QT%d' % us)
            dma(S, 'sp', KT[us][:, :], C.qkT[u * 2 + 1], [('qkT', u, 1)], [('KT', us)], 'KT%d' % us)
            dma(S, 'sp', VV[us][:, :, :], C.vS[:, u * 128:(u + 1) * 128].rearrange("(t p) j -> p t j", p=128),
                [], [('VV', us)], 'VV%d' % us)
            for hh in range(2):
                hp = 64 * hh
                P = slice(hp, hp + 64)
                if kind == 'fx':
                    fh = (u - 4) * 2 + hh
                    fs = nfx % 2
                    nfx += 1
                    dma(S, 'sp', cnq[fs][:, :], C.cneg[fh, :].partition_broadcast(128), ['cneg'], [('cnq', fs)],
                        'cnq%d' % fs)
                    dma(S, 'sp', cnk[fs][:, :], C.cneg[fh, :].rearrange("(t p) -> p t", p=128), ['cneg'],
                        [('cnk', fs)], 'cnk%d' % fs, allow_slow_non_contiguous=True)
                for qb in range(4):
                    q0 = qb * 512
                    oj = fi % 2
                    fi += 1
                    chunks = list(range(4 * qb + 3, -1, -1))
                    for ci, a in enumerate(chunks):
                        first = ci == 0
                        last = ci == len(chunks) - 1
                        b = ti % NB
                        b3 = ti % 3
                        zb = zp[ti % 2]
                        zk = ('zp', ti % 2)
                        ti += 1
                        di = a - 4 * qb
                        diag = di >= 0
                        mm(S, zb[:, :], KT[us][P, a * 128:(a + 1) * 128], QT[us][P, q0:q0 + 512], True, True,
                           [('KT', us), ('QT', us)], [zk])
                        wk = ('wT', b3)
                        wmk = ('wmT', b3)
                        if kind == 'sb':
                            msl = msb[:, 384 - di * 128:896 - di * 128] if diag else None
                            act(S, eT[b][:, :], zb[:, :], AF.Exp, [zk], [('eT', b)], scale=0.125)
                            act(S, lT[b][:, :], eT[b][:, :], AF.Ln, [('eT', b)], [('lT', b)], bias=C.one1[:, 0:1])
                            stt(S, t1T[b][:, :], zb[:, :], 0.125, lT[b][:, :], ALU.mult, ALU.subtract,
                                [zk, ('lT', b)], [('t1T', b)])
                            if diag:
                                tt(S, lmT[b][:, :], lT[b][:, :], msl, ALU.mult, [('lT', b), 'msb'], [('lmT', b)])
                                lm, lmk = lmT[b], ('lmT', b)
                            else:
                                lm, lmk = lT[b], ('lT', b)
                            mm(S, cpz[:, :], triu[:, :], lm[:, :], True, first, [lmk, 'triu'], ['cpz'])
                            if not first:
                                mm(S, cpz[:, :], C.ones_f[:, :], RT[oj][:, :], False, True, [('RT', oj)], ['cpz'])
                            tt(S, t2T[b][:, :], t1T[b][:, :], cpz[:, :], ALU.subtract, [('t1T', b), 'cpz'], [('t2T', b)])
                            act(S, wT[b3][:, :], t2T[b][:, :], AF.Exp, [('t2T', b)], [wk])
                            if diag:
                                tt(S, wmT[b3][:, :], wT[b3][:, :], msl, ALU.mult, [wk, 'msb'], [wmk])
                                wm, wmkk = wmT[b3], wmk
                            else:
                                wm, wmkk = wT[b3], wk
                            if not last:
                                if first:
                                    cp(S, 'pool', RT[oj][:, :], lm[:, :], [lmk], [('RT', oj)])
                                else:
                                    tt(S, RT[oj][:, :], RT[oj][:, :], lm[:, :], ALU.add, [('RT', oj), lmk], [('RT', oj)],
                                       eng='pool')
                        elif kind == 'fx':
                            stt(S, t1T[b][:, :], zb[:, :], 0.125, cnq[fs][:, q0:q0 + 512], ALU.mult, ALU.subtract,
                                [zk, ('cnq', fs)], [('t1T', b)])
                            act(S, wT[b3][:, :], t1T[b][:, :], AF.Exp, [('t1T', b), ('cnk', fs)], [wk],
                                bias=cnk[fs][:, a:a + 1])
                            if diag:
                                tt(S, wmT[b3][:, :], wT[b3][:, :], mfx[:, 384 - di * 128:896 - di * 128], ALU.mult,
                                   [wk, 'mfx'], [wmk])
                                wm, wmkk = wmT[b3], wmk
                            else:
                                wm, wmkk = wT[b3], wk
                        else:
                            dl = q0 - a * 128
                            act(S, wT[b3][:, :], zb[:, :], AF.Exp, [zk], [wk], scale=0.125)
                            tt(S, wmT[b3][:, :], wT[b3][:, :], mdl[:, dl + 384:dl + 896], ALU.mult, [wk, 'mdl'], [wmk])
                            wm, wmkk = wmT[b3], wmk
                        mm(S, opz[oj][P, :], VV[us][:, a, P], wm[:, :], first, last, [('VV', us), wmkk], [('opz', oj, hh)])
                        if kind != 'sb':
                            mm(S, spz[oj][P, :], C.ones_b[:, 0:64], wm[:, :], first, last, [wmkk], [('spz', oj, hh)])
                    fo = oj
                    if kind == 'sb':
                        cp(S, 'act', o32[fo][P, :], opz[oj][P, :], [('opz', oj, hh)], [('o32', fo)])
                    else:
                        S.op('dve', lambda e: e.reciprocal(out=rsm[fo][P, :], in_=spz[oj][P, :]),
                             [('spz', oj, hh)], [('rsm', fo)])
                        tt(S, o32[fo][P, :], opz[oj][P, :], rsm[fo][P, :], ALU.mult, [('opz', oj, hh), ('rsm', fo)],
                           [('o32', fo)])
                    act(S, sq[fo][P, :], o32[fo][P, :], AF.Square, [('o32', fo)], [('sq', fo)])
                    mm(S, mpz[P, :], C.ones_b[P, 0:64], sq[fo][P, :], True, True, [('sq', fo)], ['mpz'])
                    act(S, lnv[fo][P, :], mpz[P, :], AF.Ln, ['mpz'], [('lnv', fo)], bias=C.eps6[P, 0:1], scale=1.0 / 64)
                    act(S, rstd[fo][P, :], lnv[fo][P, :], AF.Exp, [('lnv', fo)], [('rstd', fo)], scale=-0.5)
                    stt(S, OT[P, u, q0:q0 + 512], o32[fo][P, :], gT[P, u:u + 1], rstd[fo][P, :], ALU.mult, ALU.mult,
                        [('o32', fo), ('rstd', fo), 'gT'], [('OT', u, hh, qb)])
    S.barrier()


def phase_m4(C, S, l, xsrc, xdst, OT):
    nc = C.nc
    with ExitStack() as es:
        Wo = _alloc(es, nc, 'Wo', [128, 16, D], BF16)
        g1 = _alloc(es, nc, 'g1', [128, D], F32)
        lg = _alloc(es, nc, 'lg1', [128, D], F32)
        lb = _alloc(es, nc, 'lb1', [128, D], F32)
        xs = [_alloc(es, nc, 'm4x%d' % i, [128, D], F32) for i in range(2)]
        yy = [_alloc(es, nc, 'm4y%d' % i, [128, D], F32) for i in range(2)]
        tmp = {'st': _alloc(es, nc, 'm4st', [128, 24], F32), 'mv': _alloc(es, nc, 'm4mv', [128, 2], F32),
               'lnv': _alloc(es, nc, 'm4lnv', [128, 1], F32), 'rstd': _alloc(es, nc, 'm4rstd', [128, 1], F32)}
        pm = [_palloc(es, nc, 'm4p%d' % i, [128, 512], F32) for i in range(8)]
        wov = C.w_out[l].rearrange("(c p) n -> p c n", p=128)
        for nb in range(4):
            dma(S, 'pool', Wo[:, :, nb * 512:(nb + 1) * 512], wov[:, :, nb * 512:(nb + 1) * 512], [], [('Wo', nb)],
                'wo%d' % nb)
        dma(S, 'sp', g1[:, :], C.modb[:, 4096:6144], [], ['g1'], 'g1')
        dma(S, 'sp', lg[:, :], C.ln1_g[l, :].partition_broadcast(128), [], ['lg'], 'lg')
        dma(S, 'sp', lb[:, :], C.ln1_b[l, :].partition_broadcast(128), [], ['lb'], 'lb')
        for t in range(NT):
            s = t % 2
            dma(S, 'sp', xs[s][:, :], xsrc[t * 128:(t + 1) * 128, :], [('xr', t)], [('m4x', s)], 'm4x%d' % s)
            for nb in range(4):
                p = s * 4 + nb
                for c in range(16):
                    mm(S, pm[p][:, :], OT[:, c, t * 128:(t + 1) * 128], Wo[:, c, nb * 512:(nb + 1) * 512],
                       c == 0, c == 15, [('Wo', nb)], [('m4p', p)])
                tt(S, yy[s][:, nb * 512:(nb + 1) * 512], pm[p][:, :], g1[:, nb * 512:(nb + 1) * 512], ALU.mult,
                   [('m4p', p), 'g1'], [('m4y', s)])
            stt(S, yy[s][:, :], xs[s][:, :], ALPHA, yy[s][:, :], ALU.mult, ALU.add, [('m4x', s), ('m4y', s)], [('m4y', s)])
            layer_norm_tile(C, S, tmp, yy[s], ('m4y', s), xs[s], ('m4x', s), lg, 'lg', lb, 'lb', 'm4ln')
            dma(S, 'sp', xdst[t * 128:(t + 1) * 128, :], xs[s][:, :], [('m4x', s)], [('xr', t)], 'm4o%d' % s)
    S.barrier()


def phase_ffn(C, S, l, xsrc, xdst):
    nc = C.nc
    for half in range(2):
        with ExitStack() as es:
            h2T = _alloc(es, nc, 'h2T', [128, 16, 1024], BF16)
            Yacc = _alloc(es, nc, 'Yacc', [128, 8, D], F32)
            gates = _alloc(es, nc, 'gates', [128, 8, NE], F32)
            with ExitStack() as e1:
                sc2 = _alloc(e1, nc, 'sc2', [128, D], F32)
                sh2 = _alloc(e1, nc, 'sh2', [128, D], F32)
                xs = [_alloc(e1, nc, 'f1x%d' % i, [128, D], F32) for i in range(2)]
                h32 = _alloc(e1, nc, 'h32', [128, D], F32)
                hbh = _alloc(e1, nc, 'hbh', [128, D], BF16)
                hbl = _alloc(e1, nc, 'hbl', [128, D], BF16)
                h2Tl = _alloc(e1, nc, 'h2Tl', [128, 16, 128], BF16)
                wr = _alloc(e1, nc, 'wr', [128, 16 * NE], F32)
                wrh = _alloc(e1, nc, 'wrh', [128, 16, NE], BF16)
                wrl = _alloc(e1, nc, 'wrl', [128, 16, NE], BF16)
                rbb = _alloc(e1, nc, 'rbb', [128, NE], F32)
                sc = _alloc(e1, nc, 'sc', [128, NE], F32)
                sel = _alloc(e1, nc, 'sel', [128, NE], F32)
                mx8 = _alloc(e1, nc, 'mx8', [128, 8], F32)
                msk = _alloc(e1, nc, 'msk', [128, NE], F32)
                sm = _alloc(e1, nc, 'sm', [128, NE], F32)
                ssum = _alloc(e1, nc, 'ssum', [128, 1], F32)
                rs = _alloc(e1, nc, 'rs', [128, 1], F32)
                tpf = [_palloc(e1, nc, 'f1tp%d' % i, [128, 1024], BF16) for i in range(2)]
                pr = _palloc(e1, nc, 'pr', [128, NE], F32)
                dma(S, 'sp', sc2[:, :], C.modb[:, 4 * 2048:5 * 2048], [], ['sc2'], 'sc2')
                dma(S, 'sp', sh2[:, :], C.modb[:, 3 * 2048:4 * 2048], [], ['sh2'], 'sh2')
                ts(S, sc2[:, :], sc2[:, :], 1.0, None, ALU.add, None, ['sc2'], ['sc2'])
                dma(S, 'sp', wr[:, :].rearrange("p (k e) -> p k e", k=16),
                    C.w_router[l].rearrange("(k p) e -> p k e", p=128), [], ['wr'], 'wr')
                cp(S, 'dve', wrh[:, :, :].rearrange("p k e -> p (k e)"), wr[:, :], ['wr'], ['wrh'])
                tt(S, wr[:, :], wr[:, :], wrh[:, :, :].rearrange("p k e -> p (k e)"), ALU.subtract, ['wr', 'wrh'], ['wr'])
                cp(S, 'dve', wrl[:, :, :].rearrange("p k e -> p (k e)"), wr[:, :], ['wr'], ['wrl'])
                dma(S, 'sp', rbb[:, :], C.router_bias[l, :].partition_broadcast(128), [], ['rbb'], 'rbb')
                S.op('pool', lambda e: e.memset(Yacc[:, :, :], 0.0), [], ['Yacc'])
                for t in range(8):
                    gt = half * 8 + t
                    s = t % 2
                    dma(S, 'sp', xs[s][:, :], xsrc[gt * 128:(gt + 1) * 128, :], [('xr', gt)], [('f1x', s)], 'f1x%d' % s)
                    tt(S, h32[:, :], xs[s][:, :], sc2[:, :], ALU.mult, [('f1x', s), 'sc2'], ['h32'])
                    tt(S, h32[:, :], h32[:, :], sh2[:, :], ALU.add, ['h32', 'sh2'], ['h32'])
                    cp(S, 'act', hbh[:, :], h32[:, :], ['h32'], ['hbh'])
                    tt(S, h32[:, :], h32[:, :], hbh[:, :], ALU.subtract, ['h32', 'hbh'], ['h32'])
                    cp(S, 'act', hbl[:, :], h32[:, :], ['h32'], ['hbl'])
                    for (src, sk, lo) in ((hbh, 'hbh', False), (hbl, 'hbl', True)):
                        for kk in range(2):
                            for k8 in range(8):
                                k = kk * 8 + k8
                                tr(S, tpf[kk][:, k8 * 128:(k8 + 1) * 128], src[:, k * 128:(k + 1) * 128], C.ident_b[:, :],
                                   [sk], [('f1tp', kk)])
                            pv = tpf[kk][:, :].rearrange("p (a b) -> p a b", a=8)
                            if lo:
                                cp(S, 'act' if kk == 0 else 'dve', h2Tl[:, kk * 8:(kk + 1) * 8, :], pv,
                                   [('f1tp', kk)], [('h2Tl', kk)])
                            else:
                                cp(S, 'act' if kk == 0 else 'dve', h2T[:, kk * 8:(kk + 1) * 8, t * 128:(t + 1) * 128], pv,
                                   [('f1tp', kk)], [('h2T', t, kk)])
                    if DEBUG.get('f1_stop', 9) < 1:
                        continue
                    n = 0
                    for k in range(KC):
                        for (lt, lk, rt, rk) in ((h2T[:, k, t * 128:(t + 1) * 128], ('h2T', t, k // 8), wrh, 'wrh'),
                                                 (h2Tl[:, k, :], ('h2Tl', k // 8), wrh, 'wrh'),
                                                 (h2T[:, k, t * 128:(t + 1) * 128], ('h2T', t, k // 8), wrl, 'wrl')):
                            mm(S, pr[:, :], lt, rt[:, k, :], n == 0, n == 3 * KC - 1, [lk, rk], ['pr'])
                            n += 1
                    act(S, sc[:, :], pr[:, :], AF.Sigmoid, ['pr'], ['sc'])
                    if DEBUG.get('f1_stop', 9) < 2:
                        continue
                    tt(S, sel[:, :], sc[:, :], rbb[:, :], ALU.add, ['sc', 'rbb'], ['sel'])
                    S.op('dve', lambda e: e.max(out=mx8[:, :], in_=sel[:, :]), ['sel'], ['mx8'])
                    if DEBUG.get('f1_stop', 9) < 3:
                        continue
                    ts(S, msk[:, :], sel[:, :], mx8[:, 5:6], None, ALU.is_ge, None, ['sel', 'mx8'], ['msk'])
                    tt(S, sm[:, :], sc[:, :], msk[:, :], ALU.mult, ['sc', 'msk'], ['sm'])
                    S.op('dve', lambda e: e.reduce_sum(out=ssum[:, :], in_=sm[:, :], axis=AX.X), ['sm'], ['ssum'])
                    S.op('dve', lambda e: e.reciprocal(out=rs[:, :], in_=ssum[:, :]), ['ssum'], ['rs'])
                    ts(S, gates[:, t, :], sm[:, :], rs[:, 0:1], 2.5, ALU.mult, ALU.mult, ['sm', 'rs'], [('gates', t)])
            S.barrier()
            with ExitStack() as e2:
                NS = DEBUG.get('NS', 5)
                ring = [_alloc(e2, nc, 'ring%d' % i, [128, 8192], BF16) for i in range(NS)]
                AT = [_alloc(e2, nc, 'AT%d' % i, [128, 4, 1024], BF16) for i in range(2)]
                sg = [_alloc(e2, nc, 'sg%d' % i, [128, 512], F32) for i in range(2)]
                pg = [_palloc(e2, nc, 'pg%d' % i, [128, 512], F32) for i in range(2)]
                pu = [_palloc(e2, nc, 'pu%d' % i, [128, 512], F32) for i in range(2)]
                py = [_palloc(e2, nc, 'py%d' % i, [128, 512], F32) for i in range(4)]
                nd = 0
                ng = 0
                nyy = 0
                for e in ([] if DEBUG.get('skip_f2') else list(range(C.ne_run)) + [NE]):
                    if e < NE:
                        wgs, wus, wds = C.w_exp_gate[l, e], C.w_exp_up[l, e], C.w_exp_down[l, e]
                    else:
                        wgs, wus, wds = C.w_sh_gate[l], C.w_sh_up[l], C.w_sh_down[l]
                    slots = []
                    for wi, wsrc in enumerate((wgs, wus, wds)):
                        rs_ = nd % NS
                        nd += 1
                        if wi < 2:
                            dst = ring[rs_][:, :].rearrange("p (k n) -> p k n", k=16)
                            src = wsrc.rearrange("(k p) n -> p k n", p=128)
                            dma(S, 'pool', dst, src, [], [('ring', rs_), ('ringb', rs_)], 'ring%d' % rs_)
                        else:
                            dst = ring[rs_][:, :].rearrange("p (k n) -> p k n", k=4)
                            src = wsrc.rearrange("(k p) n -> p k n", p=128)
                            for hf in range(2):
                                dma(S, 'pool', dst[:, :, hf * 1024:(hf + 1) * 1024], src[:, :, hf * 1024:(hf + 1) * 1024],
                                    [], [('ring', rs_) if hf == 0 else ('ringb', rs_)], 'ring%d' % rs_)
                        slots.append(rs_)
                    Wg = ring[slots[0]][:, :].rearrange("p (k n) -> p k n", k=16)
                    Wu = ring[slots[1]][:, :].rearrange("p (k n) -> p k n", k=16)
                    Wd = ring[slots[2]][:, :].rearrange("p (k n) -> p k n", k=4)
                    ab = e % 2
                    for tb in range(2):
                        for hc in range(4):
                            gb = ng % 2
                            ng += 1
                            for k in range(KC):
                                mm(S, pg[gb][:, :], Wg[:, k, hc * 128:(hc + 1) * 128], h2T[:, k, tb * 512:(tb + 1) * 512],
                                   k == 0, k == KC - 1, [('ring', slots[0])], [('pg', gb)])
                            for k in range(KC):
                                mm(S, pu[gb][:, :], Wu[:, k, hc * 128:(hc + 1) * 128], h2T[:, k, tb * 512:(tb + 1) * 512],
                                   k == 0, k == KC - 1, [('ring', slots[1])], [('pu', gb)])
                            act(S, sg[gb][:, :], pg[gb][:, :], AF.Silu, [('pg', gb)], [('sg', gb)])
                            tt(S, AT[ab][:, hc, tb * 512:(tb + 1) * 512], sg[gb][:, :], pu[gb][:, :], ALU.mult,
                               [('sg', gb), ('pu', gb)], [('AT', ab, tb)])
                    for t in range(8):
                        for nb in range(4):
                            yb = nyy % 4
                            nyy += 1
                            for hc in range(4):
                                mm(S, py[yb][:, :], AT[ab][:, hc, t * 128:(t + 1) * 128], Wd[:, hc, nb * 512:(nb + 1) * 512],
                                   hc == 0, hc == 3, [('AT', ab, t // 4), ('ring', slots[2]), ('ringb', slots[2])], [('py', yb)])
                            gsc = gates[:, t, e:e + 1] if e < NE else 1.0
                            stt(S, Yacc[:, t, nb * 512:(nb + 1) * 512], py[yb][:, :], gsc, Yacc[:, t, nb * 512:(nb + 1) * 512],
                                ALU.mult, ALU.add, [('py', yb), ('Yacc', t, nb), 'Yacc'], [('Yacc', t, nb)])
            S.barrier()
            with ExitStack() as e3:
                g2 = _alloc(e3, nc, 'g2', [128, D], F32)
                lg = _alloc(e3, nc, 'lg2', [128, D], F32)
                lb = _alloc(e3, nc, 'lb2', [128, D], F32)
                xs = [_alloc(e3, nc, 'f3x%d' % i, [128, D], F32) for i in range(2)]
                tmp = {'st': _alloc(e3, nc, 'f3st', [128, 24], F32), 'mv': _alloc(e3, nc, 'f3mv', [128, 2], F32),
                       'lnv': _alloc(e3, nc, 'f3lnv', [128, 1], F32), 'rstd': _alloc(e3, nc, 'f3rstd', [128, 1], F32)}
                dma(S, 'sp', g2[:, :], C.modb[:, 5 * 2048:6 * 2048], [], ['g2'], 'g2')
                dma(S, 'sp', lg[:, :], C.ln2_g[l, :].partition_broadcast(128), [], ['lg'], 'lg')
                dma(S, 'sp', lb[:, :], C.ln2_b[l, :].partition_broadcast(128), [], ['lb'], 'lb')
                for t in range(8):
                    gt = half * 8 + t
                    s = t % 2
                    dma(S, 'sp', xs[s][:, :], xsrc[gt * 128:(gt + 1) * 128, :], [('xr', gt)], [('f3x', s)], 'f3x%d' % s)
                    yv = Yacc[:, t, :]
                    tt(S, yv, yv, g2[:, :], ALU.mult, [('Yt', t), 'g2'], [('Yt', t)])
                    stt(S, yv, xs[s][:, :], ALPHA, yv, ALU.mult, ALU.add, [('f3x', s), ('Yt', t)], [('Yt', t)])
                    layer_norm_tile(C, S, tmp, yv, ('Yt', t), xs[s], ('f3x', s), lg, 'lg', lb, 'lb', 'f3ln')
                    dma(S, 'sp', xdst[gt * 128:(gt + 1) * 128, :], xs[s][:, :], [('f3x', s)], [('xr', gt)], 'f3o%d' % s)
            S.barrier()


def build_program(n_layers=DEPTH, stop=None, ne_decl=NE):
    nc = bass.Bass("TRN2", target_bir_lowering=False)
    C = Ctx()
    C.nc = nc

    def din(name, shape, dt=F32):
        return nc.dram_tensor(name, list(shape), dt, kind="ExternalInput").ap()

    def dscr(name, shape, dt=F32):
        return nc.dram_tensor(name, list(shape), dt, kind="Internal").ap()
    C.x = din("x", [SEQ, D])
    C.c = din("c", [16, 128])
    C.pos = din("pos", [16, 128], I32)
    C.w_ada = din("w_ada", [DEPTH, D, 6 * D])
    C.b_ada = din("b_ada", [DEPTH, 6 * D])
    C.w_in = din("w_in", [DEPTH, D, INW])
    C.b_forget = din("b_forget", [DEPTH, 12])
    C.hng = din("head_norm_g", [DEPTH, 16, 128])
    C.w_out = din("w_out", [DEPTH, D, D])
    C.ln1_g = din("ln1_g", [DEPTH, D])
    C.ln1_b = din("ln1_b", [DEPTH, D])
    C.w_router = din("w_router", [DEPTH, D, NE])
    C.router_bias = din("router_bias", [DEPTH, NE])
    C.ne_run = ne_decl
    C.w_exp_gate = din("w_exp_gate", [DEPTH, ne_decl, D, EH])
    C.w_exp_up = din("w_exp_up", [DEPTH, ne_decl, D, EH])
    C.w_exp_down = din("w_exp_down", [DEPTH, ne_decl, EH, D])
    C.w_sh_gate = din("w_sh_gate", [DEPTH, D, EH])
    C.w_sh_up = din("w_sh_up", [DEPTH, D, EH])
    C.w_sh_down = din("w_sh_down", [DEPTH, EH, D])
    C.ln2_g = din("ln2_g", [DEPTH, D])
    C.ln2_b = din("ln2_b", [DEPTH, D])
    C.cst_ident = din("cst_ident", [128, 128])
    C.cst_triu = din("cst_triu", [128, 128])
    C.cst_msb = din("cst_msb", [128, 896])
    C.cst_mfx = din("cst_mfx", [128, 896])
    C.cst_mdl = din("cst_mdl", [128, 2432])
    C.cst_invf = din("cst_invf", [128, 32])
    C.out = nc.dram_tensor("out", [SEQ, D], F32, kind="ExternalOutput").ap()
    C.xres = dscr("xres", [SEQ, D])
    C.modb = dscr("modb", [128, 6 * D])
    C.qkT = dscr("qkT", [32, 128, SEQ], BF16)
    C.vS = dscr("vS", [SEQ, D], BF16)
    C.cneg = dscr("cneg", [12, SEQ])

    S = Sched(nc)
    with ExitStack() as es:
        C.ident_f = _alloc(es, nc, 'ident_f', [128, 128], F32)
        C.ident_b = _alloc(es, nc, 'ident_b', [128, 128], BF16)
        C.ones_f = _alloc(es, nc, 'ones_f', [128, 128], F32)
        C.ones_b = _alloc(es, nc, 'ones_b', [128, 128], BF16)
        C.one1 = _alloc(es, nc, 'one1', [128, 1], F32)
        C.eps5 = _alloc(es, nc, 'eps5', [128, 1], F32)
        C.eps6 = _alloc(es, nc, 'eps6', [128, 1], F32)
        dma(S, 'sp', C.ident_f[:, :], C.cst_ident[:, :], [], ['ident_f'], 'idf')
        dma(S, 'pool', C.ident_b[:, :], C.cst_ident[:, :], [], ['ident_b'], 'idb')
        S.op('dve', lambda e: e.memset(C.ones_f[:, :], 1.0), [], ['ones_f'])
        S.op('dve', lambda e: e.memset(C.ones_b[:, :], 1.0), [], ['ones_b'])
        S.op('dve', lambda e: e.memset(C.one1[:, :], 1.0), [], ['one1'])
        S.op('dve', lambda e: e.memset(C.eps5[:, :], 1e-5), [], ['eps5'])
        S.op('dve', lambda e: e.memset(C.eps6[:, :], 1e-6), [], ['eps6'])
        S.barrier()
        for l in range(n_layers):
            xsrc = C.x if l == 0 else C.xres
            last = l == n_layers - 1
            phase_adaln(C, S, l)
            with ExitStack() as eh:
                hT = _alloc(eh, nc, 'hT', [128, 16, SEQ], BF16)
                phase_m1(C, S, l, xsrc, hT)
                phase_m2(C, S, l, hT)
            with ExitStack() as eo:
                OT = _alloc(eo, nc, 'OT', [128, 16, SEQ], BF16)
                phase_m3(C, S, l, OT)
                if stop == 'm4':
                    phase_m4(C, S, l, xsrc, C.out, OT)
                    break
                phase_m4(C, S, l, xsrc, C.xres, OT)
            phase_ffn(C, S, l, C.xres, C.out if last else C.xres)
        S.finish('sp')
    return nc


def _consts():
    p = np.arange(128)[:, None]
    u = np.arange(2432)[None, :]
    dist = u - 384 - p
    mdl = ((dist >= 0) & (dist <= 128)).astype(np.float32) \
        + ((dist >= 0) & (dist <= 512) & (dist % 4 == 0)).astype(np.float32) \
        + ((dist >= 0) & (dist <= 2048) & (dist % 16 == 0)).astype(np.float32)
    d9 = dist[:, :896]
    invf = (10000.0 ** (-np.arange(32, dtype=np.float32) / 32)).astype(np.float32)
    return {
        "cst_ident": np.eye(128, dtype=np.float32),
        "cst_triu": (p > np.arange(128)[None, :]).astype(np.float32),
        "cst_msb": (d9 > 0).astype(np.float32),
        "cst_mfx": (d9 >= 0).astype(np.float32),
        "cst_mdl": mdl.astype(np.float32),
        "cst_invf": np.ascontiguousarray(np.broadcast_to(invf[None, :], (128, 32))).astype(np.float32),
    }


def make_in_maps(inputs, cores):
    f = lambda a: np.ascontiguousarray(np.asarray(a))
    shared = {k: f(inputs[k]) for k in ("w_ada", "b_ada", "w_in", "b_forget", "w_out", "ln1_g", "ln1_b", "w_router",
                                        "router_bias", "w_exp_gate", "w_exp_up", "w_exp_down", "w_sh_gate", "w_sh_up",
                                        "w_sh_down", "ln2_g", "ln2_b")}
    shared["head_norm_g"] = f(inputs["head_norm_g"]).reshape(DEPTH, 16, 128)
    shared.update(_consts())
    x = f(inputs["x"])
    c = f(inputs["c"])
    pos = f(inputs["positions"]).astype(np.int32)
    maps = []
    for b in cores:
        m = dict(shared)
        m["x"] = x[b]
        m["c"] = c[b].reshape(16, 128)
        m["pos"] = pos[b].reshape(16, 128)
        maps.append(m)
    return maps


def kernel(**inputs):
    nc = build_program()
    in_maps = make_in_maps(inputs, list(range(N_CORES)))
    res = run_bass_kernel_spmd(nc, in_maps, core_ids=list(range(N_CORES)))
    return np.stack([np.asarray(r["out"]) for r in res.results], axis=0).astype(np.float32)
```

```python
import math
from contextlib import ExitStack
import numpy as np
import concourse.bass as bass
import concourse.mybir as mybir
from concourse.bass_utils import run_bass_kernel_spmd

F32 = mybir.dt.float32
BF16 = mybir.dt.bfloat16
I32 = mybir.dt.int32
AF = mybir.ActivationFunctionType
ALU = mybir.AluOpType
AX = mybir.AxisListType

D = 2048
SEQ = 2048
NT = 16
KC = 16
DEPTH = 2
ALPHA = float((2 * DEPTH) ** 0.25)
NE = 64
EH = 512
INW = 6156
PI = math.pi
N_CORES = 8
DEBUG = {}


class Sched:
    def __init__(self, nc):
        self.nc = nc
        self.E = {'pe': nc.tensor, 'act': nc.scalar, 'dve': nc.vector, 'pool': nc.gpsimd, 'sp': nc.sync}
        self.sem = {}
        self.cnt = {}
        for k in self.E:
            self.sem[k] = nc.semaphore('s_' + k).__enter__()
            self.cnt[k] = 0
        self.seen = {k: {} for k in self.E}
        self.res = {}
        self.dmap = {}
        self.dpool = []
        self.dfree = []

    def _phys(self, name):
        if name not in self.dmap:
            if self.dfree:
                p = self.dfree.pop()
            else:
                p = 'd%d' % len(self.dpool)
                self.dpool.append(p)
                self.sem[p] = self.nc.semaphore('s_' + p).__enter__()
                self.cnt[p] = 0
            self.dmap[name] = p
        return self.dmap[name]

    def op(self, eng, fn, reads=(), writes=(), dsem=None):
        deps = {}

        def add(tag):
            if tag is not None and deps.get(tag[0], 0) < tag[1]:
                deps[tag[0]] = tag[1]
        for r in reads:
            rec = self.res.get(r)
            if rec:
                add(rec['w'])
        for w in writes:
            rec = self.res.get(w)
            if rec:
                add(rec['w'])
                for k, v in rec['r'].items():
                    add((k, v))
        seen = self.seen[eng]
        for k, v in deps.items():
            if k == eng and eng == 'pe' and dsem is None:
                continue
            if seen.get(k, 0) >= v:
                continue
            self.E[eng].wait_ge(self.sem[k], v)
            seen[k] = v
        ins = fn(self.E[eng])
        if dsem is None:
            self.cnt[eng] += 1
            ins.then_inc(self.sem[eng], 1)
            tag = (eng, self.cnt[eng])
        else:
            p = self._phys(dsem)
            self.cnt[p] += 16
            ins.then_inc(self.sem[p], 16)
            tag = (p, self.cnt[p])
        for r in reads:
            rec = self.res.setdefault(r, {'w': None, 'r': {}})
            if rec['r'].get(tag[0], 0) < tag[1]:
                rec['r'][tag[0]] = tag[1]
        for w in writes:
            self.res[w] = {'w': tag, 'r': {}}
        return ins

    def barrier(self):
        snap = dict(self.cnt)
        for eng in self.E:
            for k, v in snap.items():
                if v > 0 and k != eng and self.seen[eng].get(k, 0) < v:
                    self.E[eng].wait_ge(self.sem[k], v)
                    self.seen[eng][k] = v
        self.res = {}
        self.dfree = list(self.dpool)
        self.dmap = {}

    def finish(self, eng='sp'):
        for k, v in self.cnt.items():
            if v > 0 and k != eng and self.seen[eng].get(k, 0) < v:
                self.E[eng].wait_ge(self.sem[k], v)
                self.seen[eng][k] = v


class Ctx:
    pass


_UID = [0]


def _alloc(es, nc, name, shape, dt):
    _UID[0] += 1
    return es.enter_context(nc.sbuf_tensor('%s_%d' % (name, _UID[0]), list(shape), dt))


def _palloc(es, nc, name, shape, dt):
    _UID[0] += 1
    return es.enter_context(nc.psum_tensor('%s_%d' % (name, _UID[0]), list(shape), dt))


def mm(S, out, lhsT, rhs, start, stop, reads, writes):
    return S.op('pe', lambda e: e.matmul(out, lhsT, rhs, start=start, stop=stop), reads, writes)


def tr(S, out, in_, ident, reads, writes):
    return S.op('pe', lambda e: e.transpose(out, in_, ident), reads, writes)


def act(S, out, in_, func, reads, writes, bias=None, scale=None):
    kw = {}
    if bias is not None:
        kw['bias'] = bias
    if scale is not None:
        kw['scale'] = scale
    return S.op('act', lambda e: e.activation(out=out, in_=in_, func=func, **kw), reads, writes)


def tt(S, out, in0, in1, op, reads, writes, eng='dve'):
    return S.op(eng, lambda e: e.tensor_tensor(out=out, in0=in0, in1=in1, op=op), reads, writes)


def ts(S, out, in0, s1, s2, op0, op1, reads, writes, eng='dve'):
    if op1 is None:
        return S.op(eng, lambda e: e.tensor_scalar(out=out, in0=in0, scalar1=s1, scalar2=None, op0=op0), reads, writes)
    return S.op(eng, lambda e: e.tensor_scalar(out=out, in0=in0, scalar1=s1, scalar2=s2, op0=op0, op1=op1), reads, writes)


def stt(S, out, in0, scalar, in1, op0, op1, reads, writes):
    return S.op('dve', lambda e: e.scalar_tensor_tensor(out=out, in0=in0, scalar=scalar, in1=in1, op0=op0, op1=op1),
                reads, writes)


def cp(S, eng, out, in_, reads, writes):
    if eng == 'act':
        return S.op('act', lambda e: e.activation(out=out, in_=in_, func=AF.Copy), reads, writes)
    return S.op(eng, lambda e: e.tensor_copy(out=out, in_=in_), reads, writes)


def dma(S, q, out, in_, reads, writes, dsem, **kw):
    if q == 'pool' and 'max_dma_last_dim' not in kw:
        kw['max_dma_last_dim'] = 4096
    return S.op(q, lambda e: e.dma_start(out=out, in_=in_, **kw), reads, writes, dsem=dsem)


def layer_norm_tile(C, S, es_t, y, yk, outt, outk, g, gk, b, bk, tag):
    t = es_t
    st, mv, lnv, rstd = t['st'], t['mv'], t['lnv'], t['rstd']
    for i in range(4):
        S.op('dve', lambda e: e.bn_stats(out=st[:, i * 6:(i + 1) * 6], in_=y[:, i * 512:(i + 1) * 512]), [yk], [(tag, 'st', i)])
    S.op('dve', lambda e: e.bn_aggr(out=mv[:, :], in_=st[:, :]), [(tag, 'st', i) for i in range(4)], [(tag, 'mv')])
    act(S, lnv[:, :], mv[:, 1:2], AF.Ln, [(tag, 'mv')], [(tag, 'lnv')], bias=C.eps5[:, 0:1])
    act(S, rstd[:, :], lnv[:, :], AF.Exp, [(tag, 'lnv')], [(tag, 'rstd')], scale=-0.5)
    ts(S, y[:, :], y[:, :], mv[:, 0:1], rstd[:, 0:1], ALU.subtract, ALU.mult, [yk, (tag, 'mv'), (tag, 'rstd')], [yk])
    tt(S, y[:, :], y[:, :], g[:, :], ALU.mult, [yk, gk], [yk])
    tt(S, outt[:, :], y[:, :], b[:, :], ALU.add, [yk, bk], [outk])


def phase_adaln(C, S, l):
    nc = C.nc
    with ExitStack() as es:
        c16 = _alloc(es, nc, 'c16', [16, 128], F32)
        cact = _alloc(es, nc, 'cact', [128, 16], F32)
        crep = _alloc(es, nc, 'crep', [128, 16, 128], F32)
        wsl = [_alloc(es, nc, 'wada%d' % i, [128, 16, 512], F32) for i in range(2)]
        brow = [_alloc(es, nc, 'brow%d' % i, [1, 512], F32) for i in range(2)]
        stg = [_alloc(es, nc, 'mstg%d' % i, [128, 512], F32) for i in range(2)]
        pT = _palloc(es, nc, 'pT', [128, 16], F32)
        pm = [_palloc(es, nc, 'pmod%d' % i, [128, 512], F32) for i in range(2)]
        dma(S, 'sp', c16[:, :], C.c[:, :], [], ['c16'], 'c16')
        tr(S, pT[:, 0:16], c16[0:16, :], C.ident_f[0:16, 0:16], ['c16'], ['pT'])
        act(S, cact[:, :], pT[:, :], AF.Silu, ['pT'], ['cact'])
        for k in range(KC):
            act(S, crep[:, k, :], C.ones_f[:, :], AF.Identity, ['cact'], [('crep', k)], scale=cact[:, k:k + 1])
        wv = C.w_ada[l].rearrange("(k p) n -> p k n", p=128)
        for j in range(24):
            s = j % 2
            dma(S, 'sp', wsl[s][:, :, :], wv[:, :, j * 512:(j + 1) * 512], [], [('wada', s)], 'wada%d' % s)
            dma(S, 'sp', brow[s][0:1, :], C.b_ada[l:l + 1, j * 512:(j + 1) * 512], [], [('brow', s)], 'brow%d' % s)
            for k in range(KC):
                mm(S, pm[s][:, :], crep[:, k, :], wsl[s][:, k, :], k == 0, False,
                   [('crep', k), ('wada', s)], [('pmod', s)])
            mm(S, pm[s][:, :], C.ones_f[0:1, :], brow[s][0:1, :], False, True, [('brow', s)], [('pmod', s)])
            cp(S, 'act' if j % 2 else 'dve', stg[s][:, :], pm[s][:, :], [('pmod', s)], [('mstg', s)])
            dma(S, 'sp', C.modb[:, j * 512:(j + 1) * 512], stg[s][:, :], [('mstg', s)], [('modb', j // 4)], 'mst%d' % s)
    S.barrier()


def phase_m1(C, S, l, xsrc, hT):
    nc = C.nc
    with ExitStack() as es:
        sc1 = _alloc(es, nc, 'sc1', [128, D], F32)
        sh1 = _alloc(es, nc, 'sh1', [128, D], F32)
        xs = [_alloc(es, nc, 'm1x%d' % i, [128, D], F32) for i in range(2)]
        hb = [_alloc(es, nc, 'm1h%d' % i, [128, D], BF16) for i in range(2)]
        tp = [_palloc(es, nc, 'm1tp%d' % i, [128, 1024], BF16) for i in range(2)]
        dma(S, 'sp', sc1[:, :], C.modb[:, 2048:4096], [], ['sc1'], 'sc1')
        dma(S, 'sp', sh1[:, :], C.modb[:, 0:2048], [], ['sh1'], 'sh1')
        ts(S, sc1[:, :], sc1[:, :], 1.0, None, ALU.add, None, ['sc1'], ['sc1'])
        for t in range(NT):
            s = t % 2
            dma(S, 'sp', xs[s][:, :], xsrc[t * 128:(t + 1) * 128, :], [('xr', t)], [('m1x', s)], 'm1x%d' % s)
            tt(S, xs[s][:, :], xs[s][:, :], sc1[:, :], ALU.mult, [('m1x', s), 'sc1'], [('m1x', s)])
            tt(S, hb[s][:, :], xs[s][:, :], sh1[:, :], ALU.add, [('m1x', s), 'sh1'], [('m1h', s)])
            for kk in range(2):
                for k8 in range(8):
                    k = kk * 8 + k8
                    tr(S, tp[kk][:, k8 * 128:(k8 + 1) * 128], hb[s][:, k * 128:(k + 1) * 128], C.ident_b[:, :],
                       [('m1h', s)], [('m1tp', kk)])
                cp(S, 'act' if kk == 0 else 'dve', hT[:, kk * 8:(kk + 1) * 8, t * 128:(t + 1) * 128],
                   tp[kk][:, :].rearrange("p (a b) -> p a b", a=8), [('m1tp', kk)], [('hT', t)])
    S.barrier()


def phase_m2(C, S, l, hT):
    nc = C.nc
    hT_all = [('hT', t) for t in range(NT)]
    with ExitStack() as es:
        NW = 3
        wsl = [_alloc(es, nc, 'm2w%d' % i, [128, 16, 512], BF16) for i in range(NW)]
        stT = [_alloc(es, nc, 'stT%d' % i, [128, SEQ], BF16) for i in range(2)]
        stV = [_alloc(es, nc, 'stV%d' % i, [128, 512], BF16) for i in range(2)]
        stD = [_alloc(es, nc, 'stD%d' % i, [128, 3, SEQ], BF16) for i in range(2)]
        cos3 = _alloc(es, nc, 'cos3', [128, 16, 6, 32], F32)
        sin3 = _alloc(es, nc, 'sin3', [128, 16, 6, 32], F32)
        ppos = _palloc(es, nc, 'm2pp', [128, 16], F32)
        with ExitStack() as er:
            posi = _alloc(er, nc, 'posi', [16, 128], I32)
            posf = _alloc(er, nc, 'posf', [16, 128], F32)
            posT = _alloc(er, nc, 'posT', [128, 16], F32)
            invf = _alloc(er, nc, 'invf', [128, 32], F32)
            ang = _alloc(er, nc, 'ang', [128, 512], F32)
            kf = _alloc(er, nc, 'kf', [128, 512], F32)
            ki = _alloc(er, nc, 'ki', [128, 512], I32)
            rr = _alloc(er, nc, 'rr', [128, 512], F32)
            wr_ = _alloc(er, nc, 'wrp', [128, 512], F32)
            sinv = _alloc(er, nc, 'sinv', [128, 512], F32)
            cosv = _alloc(er, nc, 'cosv', [128, 512], F32)
            dma(S, 'sp', posi[:, :], C.pos[:, :], [], ['posi'], 'posi')
            dma(S, 'sp', invf[:, :], C.cst_invf[:, :], [], ['invf'], 'invf')
            cp(S, 'dve', posf[:, :], posi[:, :], ['posi'], ['posf'])
            tr(S, ppos[:, 0:16], posf[0:16, :], C.ident_f[0:16, 0:16], ['posf'], ['ppos'])
            cp(S, 'dve', posT[:, :], ppos[:, :], ['ppos'], ['posT'])
            for t in range(NT):
                ts(S, ang[:, t * 32:(t + 1) * 32], invf[:, :], posT[:, t:t + 1], None, ALU.mult, None,
                   ['invf', 'posT'], ['ang'])
            ts(S, kf[:, :], ang[:, :], 1.0 / (2 * PI), None, ALU.mult, None, ['ang'], ['kf'])
            cp(S, 'dve', ki[:, :], kf[:, :], ['kf'], ['ki'])
            cp(S, 'dve', kf[:, :], ki[:, :], ['ki'], ['kf'])
            stt(S, rr[:, :], kf[:, :], -2 * PI, ang[:, :], ALU.mult, ALU.add, ['kf', 'ang'], ['rr'])
            for (shift, dst, dk) in ((0.0, sinv, 'sinv'), (PI / 2, cosv, 'cosv')):
                if shift != 0.0:
                    ts(S, rr[:, :], rr[:, :], shift, None, ALU.add, None, ['rr'], ['rr'])
                for rep in range(2):
                    ts(S, wr_[:, :], rr[:, :], PI, 2 * PI, ALU.is_gt, ALU.mult, ['rr'], ['wrp'])
                    tt(S, rr[:, :], rr[:, :], wr_[:, :], ALU.subtract, ['rr', 'wrp'], ['rr'])
                    ts(S, wr_[:, :], rr[:, :], -PI, 2 * PI, ALU.is_lt, ALU.mult, ['rr'], ['wrp'])
                    tt(S, rr[:, :], rr[:, :], wr_[:, :], ALU.add, ['rr', 'wrp'], ['rr'])
                act(S, dst[:, :], rr[:, :], AF.Sin, ['rr'], [dk])
            for h in range(6):
                cp(S, 'dve', cos3[:, :, h, :], cosv[:, :].rearrange("p (t j) -> p t j", t=16), ['cosv'], ['cos3'])
                cp(S, 'dve', sin3[:, :, h, :], sinv[:, :].rearrange("p (t j) -> p t j", t=16), ['sinv'], ['sin3'])
            S.barrier()
        ra = [_alloc(es, nc, 'ra%d' % i, [128, 6, 32], F32) for i in range(4)]
        rb = [_alloc(es, nc, 'rb%d' % i, [128, 384], BF16) for i in range(2)]
        nbf = _alloc(es, nc, 'nbf', [12, 1], F32)
        lfe = _alloc(es, nc, 'lfe', [12, SEQ], F32)
        zer = _alloc(es, nc, 'zer', [12, SEQ], F32)
        cn = _alloc(es, nc, 'cn', [12, SEQ], F32)
        pj = [_palloc(es, nc, 'm2pj%d' % i, [128, 512], F32) for i in range(4)]
        ptr = [_palloc(es, nc, 'm2pt%d' % i, [128, 512], BF16) for i in range(2)]

        wv = C.w_in[l].rearrange("(k p) n -> p k n", p=128)
        groups = [('f', 6144, 12, None)]
        for u0 in range(4):
            pass
        groups += [('fm', 0, 512, [(u, 0) for u in range(4)]), ('fm', 512, 512, [(u, 1) for u in range(4)])]
        groups += [('tv', 1024, 512, 0)]
        groups += [('fm', 1536, 384, [(4 + i, 0) for i in range(3)]), ('fm', 1920, 384, [(7 + i, 0) for i in range(3)])]
        groups += [('fm', 2304, 384, [(4 + i, 1) for i in range(3)]), ('fm', 2688, 384, [(7 + i, 1) for i in range(3)])]
        groups += [('tv', 3072, 384, 512), ('tv', 3456, 384, 896)]
        groups += [('tr', 3840, 384, [(10 + i, 0) for i in range(3)]), ('tr', 4224, 384, [(13 + i, 0) for i in range(3)])]
        groups += [('tr', 4608, 384, [(10 + i, 1) for i in range(3)]), ('tr', 4992, 384, [(13 + i, 1) for i in range(3)])]
        groups += [('tv', 5376, 384, 1280), ('tv', 5760, 384, 1664)]
        nst = 0
        npj = 0
        ntd = 0
        for gi, (kind, c0, W, info) in enumerate(groups):
            ws = gi % NW
            dma(S, 'pool', wsl[ws][:, :, 0:W], wv[:, :, c0:c0 + W], [], [('m2w', ws)], 'm2w%d' % ws)
            wk = ('m2w', ws)
            if kind == 'fm':
                for ci, (u, qk) in enumerate(info):
                    s = nst % 2
                    nst += 1
                    for tb in range(4):
                        p = npj % 4
                        npj += 1
                        for k in range(KC):
                            mm(S, pj[p][:, :], wsl[ws][:, k, ci * 128:(ci + 1) * 128], hT[:, k, tb * 512:(tb + 1) * 512],
                               k == 0, k == KC - 1, [wk] + hT_all[tb * 4:tb * 4 + 4], [('pj', p)])
                        cp(S, 'act' if tb % 2 else 'dve', stT[s][:, tb * 512:(tb + 1) * 512], pj[p][:, :],
                           [('pj', p)], [('stT', s)])
                    dma(S, 'sp', C.qkT[u * 2 + qk], stT[s][:, :], [('stT', s)], [('qkT', u, qk)], 'stT%d' % s)
            elif kind == 'tv':
                for t in range(NT):
                    s = nst % 2
                    nst += 1
                    p = npj % 4
                    npj += 1
                    for k in range(KC):
                        mm(S, pj[p][:, 0:W], hT[:, k, t * 128:(t + 1) * 128], wsl[ws][:, k, 0:W],
                           k == 0, k == KC - 1, [wk, ('hT', t)], [('pj', p)])
                    cp(S, 'act' if t % 2 else 'dve', stV[s][:, 0:W], pj[p][:, 0:W], [('pj', p)], [('stV', s)])
                    dma(S, 'sp', C.vS[t * 128:(t + 1) * 128, info:info + W], stV[s][:, 0:W], [('stV', s)],
                        [('vS', info, t)], 'stV%d' % s)
            elif kind == 'tr':
                sd = ntd % 2
                ntd += 1
                for t in range(NT):
                    p = npj % 4
                    npj += 1
                    for k in range(KC):
                        mm(S, pj[p][:, 0:W], hT[:, k, t * 128:(t + 1) * 128], wsl[ws][:, k, 0:W],
                           k == 0, k == KC - 1, [wk, ('hT', t)], [('pj', p)])
                    pv = pj[p][:, 0:384].rearrange("p (h two j) -> p h two j", h=6, two=2)
                    t1 = pv[:, :, 0, :]
                    t2 = pv[:, :, 1, :]
                    s = t % 2
                    rbv = rb[s][:, :].rearrange("p (h two j) -> p h two j", h=6, two=2)
                    tt(S, ra[0][:, :, :], t1, cos3[:, t, :, :], ALU.mult, [('pj', p), 'cos3'], ['ra0'])
                    tt(S, ra[1][:, :, :], t2, sin3[:, t, :, :], ALU.mult, [('pj', p), 'sin3'], ['ra1'])
                    tt(S, rbv[:, :, 0, :], ra[0][:, :, :], ra[1][:, :, :], ALU.subtract, ['ra0', 'ra1'], [('rb', s, 0)])
                    tt(S, ra[2][:, :, :], t1, sin3[:, t, :, :], ALU.mult, [('pj', p), 'sin3'], ['ra2'])
                    tt(S, ra[3][:, :, :], t2, cos3[:, t, :, :], ALU.mult, [('pj', p), 'cos3'], ['ra3'])
                    tt(S, rbv[:, :, 1, :], ra[2][:, :, :], ra[3][:, :, :], ALU.add, ['ra2', 'ra3'], [('rb', s, 1)])
                    for c in range(3):
                        tr(S, ptr[s][:, c * 128:(c + 1) * 128], rb[s][:, c * 128:(c + 1) * 128], C.ident_b[:, :],
                           [('rb', s, 0), ('rb', s, 1)], [('ptr', s)])
                    cp(S, 'act', stD[sd][:, :, t * 128:(t + 1) * 128],
                       ptr[s][:, 0:384].rearrange("p (c j) -> p c j", c=3), [('ptr', s)], [('stD', sd)])
                for c, (u, qk) in enumerate(info):
                    dma(S, 'sp', C.qkT[u * 2 + qk], stD[sd][:, c, :], [('stD', sd)], [('qkT', u, qk)],
                        'stD%d_%d' % (sd, c))
            else:
                dma(S, 'sp', nbf[:, :], C.b_forget[l:l + 1, :].rearrange("o h -> h o"), [], ['nbf'], 'nbf',
                    allow_slow_non_contiguous=True)
                ts(S, nbf[:, :], nbf[:, :], -1.0, None, ALU.mult, None, ['nbf'], ['nbf'])
                S.op('dve', lambda e: e.memset(zer[:, :], 0.0), [], ['zer'])
                for tb in range(4):
                    p = npj % 4
                    npj += 1
                    for k in range(KC):
                        mm(S, pj[p][0:12, :], wsl[ws][:, k, 0:12], hT[:, k, tb * 512:(tb + 1) * 512],
                           k == 0, k == KC - 1, [wk] + hT_all[tb * 4:tb * 4 + 4], [('pj', p)])
                    act(S, lfe[:, tb * 512:(tb + 1) * 512], pj[p][0:12, :], AF.Exp, [('pj', p), 'nbf'], [('lfe', tb)],
                        bias=nbf[:, 0:1], scale=-1.0)
                    act(S, lfe[:, tb * 512:(tb + 1) * 512], lfe[:, tb * 512:(tb + 1) * 512], AF.Ln,
                        [('lfe', tb)], [('lfe', tb)], bias=C.one1[0:12, 0:1])
                S.op('dve', lambda e: e.tensor_tensor_scan(out=cn[:, :], data0=lfe[:, :], data1=zer[:, :], initial=0.0,
                                                           op0=ALU.add, op1=ALU.add),
                     [('lfe', tb) for tb in range(4)] + ['zer'], ['cn'])
                dma(S, 'sp', C.cneg[:, :], cn[:, :], ['cn'], ['cneg'], 'cn')
    S.barrier()


def phase_m3(C, S, l, OT):
    nc = C.nc
    with ExitStack() as es:
        msb = _alloc(es, nc, 'msb', [128, 896], BF16)
        mfx = _alloc(es, nc, 'mfx', [128, 896], BF16)
        mdl = _alloc(es, nc, 'mdl', [128, 2432], BF16)
        triu = _alloc(es, nc, 'triu', [128, 128], F32)
        g16 = _alloc(es, nc, 'g16', [16, 128], F32)
        gT = _alloc(es, nc, 'gT', [128, 16], F32)
        QT = [_alloc(es, nc, 'QT%d' % i, [128, SEQ], BF16) for i in range(2)]
        KT = [_alloc(es, nc, 'KT%d' % i, [128, SEQ], BF16) for i in range(2)]
        VV = [_alloc(es, nc, 'VV%d' % i, [128, 16, 128], BF16) for i in range(2)]
        cnq = [_alloc(es, nc, 'cnq%d' % i, [128, SEQ], F32) for i in range(2)]
        cnk = [_alloc(es, nc, 'cnk%d' % i, [128, 16], F32) for i in range(2)]
        eT = [_alloc(es, nc, 'eT%d' % i, [128, 512], F32) for i in range(2)]
        lT = [_alloc(es, nc, 'lT%d' % i, [128, 512], F32) for i in range(3)]
        t1T = [_alloc(es, nc, 't1T%d' % i, [128, 512], F32) for i in range(3)]
        t2T = [_alloc(es, nc, 't2T%d' % i, [128, 512], F32) for i in range(2)]
        lmT = [_alloc(es, nc, 'lmT%d' % i, [128, 512], F32) for i in range(2)]
        RT = [_alloc(es, nc, 'RT%d' % i, [128, 512], F32) for i in range(2)]
        wT = [_alloc(es, nc, 'wT%d' % i, [128, 512], BF16) for i in range(3)]
        wmT = [_alloc(es, nc, 'wmT%d' % i, [128, 512], BF16) for i in range(3)]
        o32 = [_alloc(es, nc, 'o32_%d' % i, [128, 512], F32) for i in range(2)]
        rsm = [_alloc(es, nc, 'rsm%d' % i, [128, 512], F32) for i in range(2)]
        sq = [_alloc(es, nc, 'sq%d' % i, [128, 512], BF16) for i in range(2)]
        lnv = [_alloc(es, nc, 'lnv%d' % i, [128, 512], F32) for i in range(2)]
        rstd = [_alloc(es, nc, 'rstd%d' % i, [128, 512], F32) for i in range(2)]
        zp = [_palloc(es, nc, 'zp%d' % i, [128, 512], F32) for i in range(3)]
        aux = [_palloc(es, nc, 'aux%d' % i, [128, 512], F32) for i in range(2)]
        opz = [_palloc(es, nc, 'opz%d' % i, [128, 512], F32) for i in range(2)]
        mpz = _palloc(es, nc, 'mpz', [128, 512], F32)

        dma(S, 'pool', msb[:, :], C.cst_msb[:, :], [], ['msb'], 'msb')
        dma(S, 'pool', mfx[:, :], C.cst_mfx[:, :], [], ['mfx'], 'mfx')
        dma(S, 'pool', mdl[:, :], C.cst_mdl[:, :], [], ['mdl'], 'mdl')
        dma(S, 'sp', triu[:, :], C.cst_triu[:, :], [], ['triu'], 'triu')
        dma(S, 'sp', g16[:, :], C.hng[l], [], ['g16'], 'g16')
        tr(S, mpz[:, 0:16], g16[0:16, :], C.ident_f[0:16, 0:16], ['g16'], ['mpz'])
        cp(S, 'dve', gT[:, :], mpz[:, 0:16], ['mpz'], ['gT'])

        NST = 12
        tiles = []

        def unit_load(u, us):
            def f():
                dma(S, 'sp', QT[us][:, :], C.qkT[u * 2 + 0], [('qkT', u, 0)], [('QT', us)], 'QT%d' % us)
                dma(S, 'sp', KT[us][:, :], C.qkT[u * 2 + 1], [('qkT', u, 1)], [('KT', us)], 'KT%d' % us)
                dma(S, 'sp', VV[us][:, :, :], C.vS[:, u * 128:(u + 1) * 128].rearrange("(t p) j -> p t j", p=128),
                    [], [('VV', us)], 'VV%d' % us)
            return f

        def fox_load(fh, fs):
            def f():
                dma(S, 'sp', cnq[fs][:, :], C.cneg[fh, :].partition_broadcast(128), ['cneg'], [('cnq', fs)],
                    'cnq%d' % fs)
                dma(S, 'sp', cnk[fs][:, :], C.cneg[fh, :].rearrange("(t p) -> p t", p=128), ['cneg'],
                    [('cnk', fs)], 'cnk%d' % fs, allow_slow_non_contiguous=True)
            return f

        def make_final(kind, u, hh, qb, oj, fo):
            hp = 64 * hh
            P = slice(hp, hp + 64)
            q0 = qb * 512

            def f():
                if kind == 'sb':
                    cp(S, 'act', o32[fo][P, :], opz[oj][P, :], [('opz', oj, hh)], [('o32', fo)])
                else:
                    S.op('dve', lambda e: e.reciprocal(out=rsm[fo][P, :], in_=aux[oj][P, :]),
                         [('aux', oj, hh)], [('rsm', fo)])
                    tt(S, o32[fo][P, :], opz[oj][P, :], rsm[fo][P, :], ALU.mult, [('opz', oj, hh), ('rsm', fo)],
                       [('o32', fo)])
                act(S, sq[fo][P, :], o32[fo][P, :], AF.Square, [('o32', fo)], [('sq', fo)])
                mm(S, mpz[P, :], C.ones_b[P, 0:64], sq[fo][P, :], True, True, [('sq', fo)], ['mpz'])
                act(S, lnv[fo][P, :], mpz[P, :], AF.Ln, ['mpz'], [('lnv', fo)], bias=C.eps6[P, 0:1], scale=1.0 / 64)
                act(S, rstd[fo][P, :], lnv[fo][P, :], AF.Exp, [('lnv', fo)], [('rstd', fo)], scale=-0.5)
                stt(S, OT[P, u, q0:q0 + 512], o32[fo][P, :], gT[P, u:u + 1], rstd[fo][P, :], ALU.mult, ALU.mult,
                    [('o32', fo), ('rstd', fo), 'gT'], [('OT', u, hh, qb)])
            return f

        def make_tile(kind, u, us, hh, qb, a, first, last, ti, oj, fs, pre, fin):
            hp = 64 * hh
            P = slice(hp, hp + 64)
            q0 = qb * 512
            di = a - 4 * qb
            diag = di >= 0
            z = zp[ti % 3]
            zk = ('zp', ti % 3)
            b2 = ti % 2
            b3 = ti % 3
            st = [None] * NST

            def s_qk():
                for p_ in pre:
                    p_()
                mm(S, z[:, :], KT[us][P, a * 128:(a + 1) * 128], QT[us][P, q0:q0 + 512], True, True,
                   [('KT', us), ('QT', us)], [zk])

            def s_pv(wm, wmkk, with_sum):
                def f():
                    mm(S, opz[oj][P, :], VV[us][:, a, P], wm[:, :], first, last, [('VV', us), wmkk], [('opz', oj, hh)])
                    if with_sum:
                        mm(S, aux[oj][P, :], C.ones_b[:, 0:64], wm[:, :], first, last, [wmkk], [('aux', oj, hh)])
                return f
            st[0] = s_qk
            if kind == 'sb':
                msl = msb[:, 384 - di * 128:896 - di * 128] if diag else None
                lm, lmk = (lmT[b2], ('lmT', b2)) if diag else (lT[b3], ('lT', b3))
                wm, wmkk = (wmT[b3], ('wmT', b3)) if diag else (wT[b3], ('wT', b3))
                ca = aux[ti % 2]
                cak = ('auxc', ti % 2)

                def s1():
                    act(S, eT[b2][:, :], z[:, :], AF.Exp, [zk], [('eT', b2)], scale=0.125)
                    act(S, lT[b3][:, :], eT[b2][:, :], AF.Ln, [('eT', b2)], [('lT', b3)], bias=C.one1[:, 0:1])

                def s2():
                    stt(S, t1T[b3][:, :], z[:, :], 0.125, lT[b3][:, :], ALU.mult, ALU.subtract,
                        [zk, ('lT', b3)], [('t1T', b3)])
                    if diag:
                        tt(S, lmT[b2][:, :], lT[b3][:, :], msl, ALU.mult, [('lT', b3), 'msb'], [('lmT', b2)])

                def s3():
                    mm(S, ca[:, :], triu[:, :], lm[:, :], True, first, [lmk, 'triu'], [cak])
                    if not first:
                        mm(S, ca[:, :], C.ones_f[:, :], RT[oj][:, :], False, True, [('RT', oj)], [cak])
                    if not last:
                        if first:
                            cp(S, 'pool', RT[oj][:, :], lm[:, :], [lmk], [('RT', oj)])
                        else:
                            tt(S, RT[oj][:, :], RT[oj][:, :], lm[:, :], ALU.add, [('RT', oj), lmk], [('RT', oj)],
                               eng='pool')

                def s4():
                    tt(S, t2T[b2][:, :], t1T[b3][:, :], ca[:, :], ALU.subtract, [('t1T', b3), cak], [('t2T', b2)])

                def s5():
                    act(S, wT[b3][:, :], t2T[b2][:, :], AF.Exp, [('t2T', b2)], [('wT', b3)])

                def s6():
                    if diag:
                        tt(S, wmT[b3][:, :], wT[b3][:, :], msl, ALU.mult, [('wT', b3), 'msb'], [('wmT', b3)], eng='pool')
                st[1], st[2], st[3], st[4], st[5], st[6] = s1, s2, s3, s4, s5, s6
                st[7] = s_pv(wm, wmkk, False)
                if fin is not None:
                    st[8] = fin
            elif kind == 'fx':
                wm, wmkk = (wmT[b3], ('wmT', b3)) if diag else (wT[b3], ('wT', b3))

                def s1():
                    stt(S, t1T[b3][:, :], z[:, :], 0.125, cnq[fs][:, q0:q0 + 512], ALU.mult, ALU.subtract,
                        [zk, ('cnq', fs)], [('t1T', b3)])

                def s2():
                    act(S, wT[b3][:, :], t1T[b3][:, :], AF.Exp, [('t1T', b3), ('cnk', fs)], [('wT', b3)],
                        bias=cnk[fs][:, a:a + 1])

                def s3():
                    if diag:
                        tt(S, wmT[b3][:, :], wT[b3][:, :], mfx[:, 384 - di * 128:896 - di * 128], ALU.mult,
                           [('wT', b3), 'mfx'], [('wmT', b3)], eng='pool')
                st[1], st[2], st[3] = s1, s2, s3
                st[4] = s_pv(wm, wmkk, True)
                if fin is not None:
                    st[5] = fin
            else:
                dl = q0 - a * 128

                def s1():
                    act(S, wT[b3][:, :], z[:, :], AF.Exp, [zk], [('wT', b3)], scale=0.125)

                def s2():
                    tt(S, wmT[b3][:, :], wT[b3][:, :], mdl[:, dl + 384:dl + 896], ALU.mult, [('wT', b3), 'mdl'],
                       [('wmT', b3)])
                st[1], st[2] = s1, s2
                st[3] = s_pv(wmT[b3], ('wmT', b3), True)
                if fin is not None:
                    st[4] = fin
            return st

        def emit(tl):
            n = len(tl)
            for step in range(n + NST):
                for k in range(NST):
                    i = step - k
                    if 0 <= i < n and tl[i][k] is not None:
                        tl[i][k]()

        ti = 0
        fi = 0
        nfx = 0
        for u in range(16):
            kind = 'sb' if u < 4 else ('fx' if u < 10 else 'dl')
            if u in (4, 10):
                emit(tiles)
                tiles = []
            us = u % 2
            pre_u = [unit_load(u, us)]
            for hh in range(2):
                fs = None
                pre_h = []
                if kind == 'fx':
                    fs = nfx % 2
                    pre_h = [fox_load((u - 4) * 2 + hh, fs)]
                    nfx += 1
                for qb in range(4):
                    oj = fi % 2
                    fo = fi % 2
                    fi += 1
                    chunks = list(range(4 * qb + 3, -1, -1))
                    for ci, a in enumerate(chunks):
                        first = ci == 0
                        last = ci == len(chunks) - 1
                        pre = []
                        if first and qb == 0:
                            pre = (pre_u if hh == 0 else []) + pre_h
                        fin = make_final(kind, u, hh, qb, oj, fo) if last else None
                        tiles.append(make_tile(kind, u, us, hh, qb, a, first, last, ti, oj, fs, pre, fin))
                        ti += 1
        emit(tiles)
    S.barrier()


def phase_m4(C, S, l, xsrc, xdst, OT):
    nc = C.nc
    with ExitStack() as es:
        Wo = _alloc(es, nc, 'Wo', [128, 16, D], BF16)
        g1 = _alloc(es, nc, 'g1', [128, D], F32)
        lg = _alloc(es, nc, 'lg1', [128, D], F32)
        lb = _alloc(es, nc, 'lb1', [128, D], F32)
        xs = [_alloc(es, nc, 'm4x%d' % i, [128, D], F32) for i in range(2)]
        yy = [_alloc(es, nc, 'm4y%d' % i, [128, D], F32) for i in range(2)]
        tmp = {'st': _alloc(es, nc, 'm4st', [128, 24], F32), 'mv': _alloc(es, nc, 'm4mv', [128, 2], F32),
               'lnv': _alloc(es, nc, 'm4lnv', [128, 1], F32), 'rstd': _alloc(es, nc, 'm4rstd', [128, 1], F32)}
        pm = [_palloc(es, nc, 'm4p%d' % i, [128, 512], F32) for i in range(8)]
        wov = C.w_out[l].rearrange("(c p) n -> p c n", p=128)
        for nb in range(4):
            dma(S, 'pool', Wo[:, :, nb * 512:(nb + 1) * 512], wov[:, :, nb * 512:(nb + 1) * 512], [], [('Wo', nb)],
                'wo%d' % nb)
        dma(S, 'sp', g1[:, :], C.modb[:, 4096:6144], [], ['g1'], 'g1')
        dma(S, 'sp', lg[:, :], C.ln1_g[l, :].partition_broadcast(128), [], ['lg'], 'lg')
        dma(S, 'sp', lb[:, :], C.ln1_b[l, :].partition_broadcast(128), [], ['lb'], 'lb')
        for t in range(NT):
            s = t % 2
            dma(S, 'sp', xs[s][:, :], xsrc[t * 128:(t + 1) * 128, :], [('xr', t)], [('m4x', s)], 'm4x%d' % s)
            for nb in range(4):
                p = s * 4 + nb
                for c in range(16):
                    mm(S, pm[p][:, :], OT[:, c, t * 128:(t + 1) * 128], Wo[:, c, nb * 512:(nb + 1) * 512],
                       c == 0, c == 15, [('Wo', nb)], [('m4p', p)])
                tt(S, yy[s][:, nb * 512:(nb + 1) * 512], pm[p][:, :], g1[:, nb * 512:(nb + 1) * 512], ALU.mult,
                   [('m4p', p), 'g1'], [('m4y', s)])
            stt(S, yy[s][:, :], xs[s][:, :], ALPHA, yy[s][:, :], ALU.mult, ALU.add, [('m4x', s), ('m4y', s)], [('m4y', s)])
            layer_norm_tile(C, S, tmp, yy[s], ('m4y', s), xs[s], ('m4x', s), lg, 'lg', lb, 'lb', 'm4ln')
            dma(S, 'sp', xdst[t * 128:(t + 1) * 128, :], xs[s][:, :], [('m4x', s)], [('xr', t)], 'm4o%d' % s)
    S.barrier()


def phase_ffn(C, S, l, xsrc, xdst):
    nc = C.nc
    for half in range(2):
        with ExitStack() as es:
            h2T = _alloc(es, nc, 'h2T', [128, 16, 1024], BF16)
            Yacc = _alloc(es, nc, 'Yacc', [128, 8, D], F32)
            gates = _alloc(es, nc, 'gates', [128, 8, NE], F32)
            with ExitStack() as e1:
                sc2 = _alloc(e1, nc, 'sc2', [128, D], F32)
                sh2 = _alloc(e1, nc, 'sh2', [128, D], F32)
                xs = [_alloc(e1, nc, 'f1x%d' % i, [128, D], F32) for i in range(2)]
                h32 = _alloc(e1, nc, 'h32', [128, D], F32)
                hbh = _alloc(e1, nc, 'hbh', [128, D], BF16)
                hbl = _alloc(e1, nc, 'hbl', [128, D], BF16)
                h2Tl = _alloc(e1, nc, 'h2Tl', [128, 16, 128], BF16)
                wr = _alloc(e1, nc, 'wr', [128, 16 * NE], F32)
                wrh = _alloc(e1, nc, 'wrh', [128, 16, NE], BF16)
                wrl = _alloc(e1, nc, 'wrl', [128, 16, NE], BF16)
                rbb = _alloc(e1, nc, 'rbb', [128, NE], F32)
                sc = _alloc(e1, nc, 'sc', [128, NE], F32)
                sel = _alloc(e1, nc, 'sel', [128, NE], F32)
                mx8 = _alloc(e1, nc, 'mx8', [128, 8], F32)
                msk = _alloc(e1, nc, 'msk', [128, NE], F32)
                sm = _alloc(e1, nc, 'sm', [128, NE], F32)
                ssum = _alloc(e1, nc, 'ssum', [128, 1], F32)
                rs = _alloc(e1, nc, 'rs', [128, 1], F32)
                tpf = [_palloc(e1, nc, 'f1tp%d' % i, [128, 1024], BF16) for i in range(2)]
                pr = _palloc(e1, nc, 'pr', [128, NE], F32)
                dma(S, 'sp', sc2[:, :], C.modb[:, 4 * 2048:5 * 2048], [], ['sc2'], 'sc2')
                dma(S, 'sp', sh2[:, :], C.modb[:, 3 * 2048:4 * 2048], [], ['sh2'], 'sh2')
                ts(S, sc2[:, :], sc2[:, :], 1.0, None, ALU.add, None, ['sc2'], ['sc2'])
                dma(S, 'sp', wr[:, :].rearrange("p (k e) -> p k e", k=16),
                    C.w_router[l].rearrange("(k p) e -> p k e", p=128), [], ['wr'], 'wr')
                cp(S, 'dve', wrh[:, :, :].rearrange("p k e -> p (k e)"), wr[:, :], ['wr'], ['wrh'])
                tt(S, wr[:, :], wr[:, :], wrh[:, :, :].rearrange("p k e -> p (k e)"), ALU.subtract, ['wr', 'wrh'], ['wr'])
                cp(S, 'dve', wrl[:, :, :].rearrange("p k e -> p (k e)"), wr[:, :], ['wr'], ['wrl'])
                dma(S, 'sp', rbb[:, :], C.router_bias[l, :].partition_broadcast(128), [], ['rbb'], 'rbb')
                S.op('pool', lambda e: e.memset(Yacc[:, :, :], 0.0), [], ['Yacc'])
                for t in range(8):
                    gt = half * 8 + t
                    s = t % 2
                    dma(S, 'sp', xs[s][:, :], xsrc[gt * 128:(gt + 1) * 128, :], [('xr', gt)], [('f1x', s)], 'f1x%d' % s)
                    tt(S, h32[:, :], xs[s][:, :], sc2[:, :], ALU.mult, [('f1x', s), 'sc2'], ['h32'])
                    tt(S, h32[:, :], h32[:, :], sh2[:, :], ALU.add, ['h32', 'sh2'], ['h32'])
                    cp(S, 'act', hbh[:, :], h32[:, :], ['h32'], ['hbh'])
                    tt(S, h32[:, :], h32[:, :], hbh[:, :], ALU.subtract, ['h32', 'hbh'], ['h32'])
                    cp(S, 'act', hbl[:, :], h32[:, :], ['h32'], ['hbl'])
                    for (src, sk, lo) in ((hbh, 'hbh', False), (hbl, 'hbl', True)):
                        for kk in range(2):
                            for k8 in range(8):
                                k = kk * 8 + k8
                                tr(S, tpf[kk][:, k8 * 128:(k8 + 1) * 128], src[:, k * 128:(k + 1) * 128], C.ident_b[:, :],
                                   [sk], [('f1tp', kk)])
                            pv = tpf[kk][:, :].rearrange("p (a b) -> p a b", a=8)
                            if lo:
                                cp(S, 'act' if kk == 0 else 'dve', h2Tl[:, kk * 8:(kk + 1) * 8, :], pv,
                                   [('f1tp', kk)], [('h2Tl', kk)])
                            else:
                                cp(S, 'act' if kk == 0 else 'dve', h2T[:, kk * 8:(kk + 1) * 8, t * 128:(t + 1) * 128], pv,
                                   [('f1tp', kk)], [('h2T', t, kk)])
                    if DEBUG.get('f1_stop', 9) < 1:
                        continue
                    n = 0
                    for k in range(KC):
                        for (lt, lk, rt, rk) in ((h2T[:, k, t * 128:(t + 1) * 128], ('h2T', t, k // 8), wrh, 'wrh'),
                                                 (h2Tl[:, k, :], ('h2Tl', k // 8), wrh, 'wrh'),
                                                 (h2T[:, k, t * 128:(t + 1) * 128], ('h2T', t, k // 8), wrl, 'wrl')):
                            mm(S, pr[:, :], lt, rt[:, k, :], n == 0, n == 3 * KC - 1, [lk, rk], ['pr'])
                            n += 1
                    act(S, sc[:, :], pr[:, :], AF.Sigmoid, ['pr'], ['sc'])
                    if DEBUG.get('f1_stop', 9) < 2:
                        continue
                    tt(S, sel[:, :], sc[:, :], rbb[:, :], ALU.add, ['sc', 'rbb'], ['sel'])
                    S.op('dve', lambda e: e.max(out=mx8[:, :], in_=sel[:, :]), ['sel'], ['mx8'])
                    if DEBUG.get('f1_stop', 9) < 3:
                        continue
                    ts(S, msk[:, :], sel[:, :], mx8[:, 5:6], None, ALU.is_ge, None, ['sel', 'mx8'], ['msk'])
                    tt(S, sm[:, :], sc[:, :], msk[:, :], ALU.mult, ['sc', 'msk'], ['sm'])
                    S.op('dve', lambda e: e.reduce_sum(out=ssum[:, :], in_=sm[:, :], axis=AX.X), ['sm'], ['ssum'])
                    S.op('dve', lambda e: e.reciprocal(out=rs[:, :], in_=ssum[:, :]), ['ssum'], ['rs'])
                    ts(S, gates[:, t, :], sm[:, :], rs[:, 0:1], 2.5, ALU.mult, ALU.mult, ['sm', 'rs'], [('gates', t)])
            S.barrier()
            with ExitStack() as e2:
                NS = DEBUG.get('NS', 5)
                ring = [_alloc(e2, nc, 'ring%d' % i, [128, 8192], BF16) for i in range(NS)]
                AT = [_alloc(e2, nc, 'AT%d' % i, [128, 4, 1024], BF16) for i in range(2)]
                sg = [_alloc(e2, nc, 'sg%d' % i, [128, 512], F32) for i in range(2)]
                pg = [_palloc(e2, nc, 'pg%d' % i, [128, 512], F32) for i in range(2)]
                pu = [_palloc(e2, nc, 'pu%d' % i, [128, 512], F32) for i in range(2)]
                py = [_palloc(e2, nc, 'py%d' % i, [128, 512], F32) for i in range(4)]
                nd = 0
                ng = 0
                nyy = 0
                for e in ([] if DEBUG.get('skip_f2') else list(range(C.ne_run)) + [NE]):
                    if e < NE:
                        wgs, wus, wds = C.w_exp_gate[l, e], C.w_exp_up[l, e], C.w_exp_down[l, e]
                    else:
                        wgs, wus, wds = C.w_sh_gate[l], C.w_sh_up[l], C.w_sh_down[l]
                    slots = []
                    for wi, wsrc in enumerate((wgs, wus, wds)):
                        rs_ = nd % NS
                        nd += 1
                        if wi < 2:
                            dst = ring[rs_][:, :].rearrange("p (k n) -> p k n", k=16)
                            src = wsrc.rearrange("(k p) n -> p k n", p=128)
                            dma(S, 'pool', dst, src, [], [('ring', rs_), ('ringb', rs_)], 'ring%d' % rs_)
                        else:
                            dst = ring[rs_][:, :].rearrange("p (k n) -> p k n", k=4)
                            src = wsrc.rearrange("(k p) n -> p k n", p=128)
                            for hf in range(2):
                                dma(S, 'pool', dst[:, :, hf * 1024:(hf + 1) * 1024], src[:, :, hf * 1024:(hf + 1) * 1024],
                                    [], [('ring', rs_) if hf == 0 else ('ringb', rs_)], 'ring%d' % rs_)
                        slots.append(rs_)
                    Wg = ring[slots[0]][:, :].rearrange("p (k n) -> p k n", k=16)
                    Wu = ring[slots[1]][:, :].rearrange("p (k n) -> p k n", k=16)
                    Wd = ring[slots[2]][:, :].rearrange("p (k n) -> p k n", k=4)
                    ab = e % 2
                    for tb in range(2):
                        for hc in range(4):
                            gb = ng % 2
                            ng += 1
                            for k in range(KC):
                                mm(S, pg[gb][:, :], Wg[:, k, hc * 128:(hc + 1) * 128], h2T[:, k, tb * 512:(tb + 1) * 512],
                                   k == 0, k == KC - 1, [('ring', slots[0])], [('pg', gb)])
                            for k in range(KC):
                                mm(S, pu[gb][:, :], Wu[:, k, hc * 128:(hc + 1) * 128], h2T[:, k, tb * 512:(tb + 1) * 512],
                                   k == 0, k == KC - 1, [('ring', slots[1])], [('pu', gb)])
                            act(S, sg[gb][:, :], pg[gb][:, :], AF.Silu, [('pg', gb)], [('sg', gb)])
                            tt(S, AT[ab][:, hc, tb * 512:(tb + 1) * 512], sg[gb][:, :], pu[gb][:, :], ALU.mult,
                               [('sg', gb), ('pu', gb)], [('AT', ab, tb)])
                    for t in range(8):
                        for nb in range(4):
                            yb = nyy % 4
                            nyy += 1
                            for hc in range(4):
                                mm(S, py[yb][:, :], AT[ab][:, hc, t * 128:(t + 1) * 128], Wd[:, hc, nb * 512:(nb + 1) * 512],
                                   hc == 0, hc == 3, [('AT', ab, t // 4), ('ring', slots[2]), ('ringb', slots[2])], [('py', yb)])
                            gsc = gates[:, t, e:e + 1] if e < NE else 1.0
                            stt(S, Yacc[:, t, nb * 512:(nb + 1) * 512], py[yb][:, :], gsc, Yacc[:, t, nb * 512:(nb + 1) * 512],
                                ALU.mult, ALU.add, [('py', yb), ('Yacc', t, nb), 'Yacc'], [('Yacc', t, nb)])
            S.barrier()
            with ExitStack() as e3:
                g2 = _alloc(e3, nc, 'g2', [128, D], F32)
                lg = _alloc(e3, nc, 'lg2', [128, D], F32)
                lb = _alloc(e3, nc, 'lb2', [128, D], F32)
                xs = [_alloc(e3, nc, 'f3x%d' % i, [128, D], F32) for i in range(2)]
                tmp = {'st': _alloc(e3, nc, 'f3st', [128, 24], F32), 'mv': _alloc(e3, nc, 'f3mv', [128, 2], F32),
                       'lnv': _alloc(e3, nc, 'f3lnv', [128, 1], F32), 'rstd': _alloc(e3, nc, 'f3rstd', [128, 1], F32)}
                dma(S, 'sp', g2[:, :], C.modb[:, 5 * 2048:6 * 2048], [], ['g2'], 'g2')
                dma(S, 'sp', lg[:, :], C.ln2_g[l, :].partition_broadcast(128), [], ['lg'], 'lg')
                dma(S, 'sp', lb[:, :], C.ln2_b[l, :].partition_broadcast(128), [], ['lb'], 'lb')
                for t in range(8):
                    gt = half * 8 + t
                    s = t % 2
                    dma(S, 'sp', xs[s][:, :], xsrc[gt * 128:(gt + 1) * 128, :], [('xr', gt)], [('f3x', s)], 'f3x%d' % s)
                    yv = Yacc[:, t, :]
                    tt(S, yv, yv, g2[:, :], ALU.mult, [('Yt', t), 'g2'], [('Yt', t)])
                    stt(S, yv, xs[s][:, :], ALPHA, yv, ALU.mult, ALU.add, [('f3x', s), ('Yt', t)], [('Yt', t)])
                    layer_norm_tile(C, S, tmp, yv, ('Yt', t), xs[s], ('f3x', s), lg, 'lg', lb, 'lb', 'f3ln')
                    dma(S, 'sp', xdst[gt * 128:(gt + 1) * 128, :], xs[s][:, :], [('f3x', s)], [('xr', gt)], 'f3o%d' % s)
            S.barrier()


def build_program(n_layers=DEPTH, stop=None, ne_decl=NE):
    nc = bass.Bass("TRN2", target_bir_lowering=False)
    C = Ctx()
    C.nc = nc

    def din(name, shape, dt=F32):
        return nc.dram_tensor(name, list(shape), dt, kind="ExternalInput").ap()

    def dscr(name, shape, dt=F32):
        return nc.dram_tensor(name, list(shape), dt, kind="Internal").ap()
    C.x = din("x", [SEQ, D])
    C.c = din("c", [16, 128])
    C.pos = din("pos", [16, 128], I32)
    C.w_ada = din("w_ada", [DEPTH, D, 6 * D])
    C.b_ada = din("b_ada", [DEPTH, 6 * D])
    C.w_in = din("w_in", [DEPTH, D, INW])
    C.b_forget = din("b_forget", [DEPTH, 12])
    C.hng = din("head_norm_g", [DEPTH, 16, 128])
    C.w_out = din("w_out", [DEPTH, D, D])
    C.ln1_g = din("ln1_g", [DEPTH, D])
    C.ln1_b = din("ln1_b", [DEPTH, D])
    C.w_router = din("w_router", [DEPTH, D, NE])
    C.router_bias = din("router_bias", [DEPTH, NE])
    C.ne_run = ne_decl
    C.w_exp_gate = din("w_exp_gate", [DEPTH, ne_decl, D, EH])
    C.w_exp_up = din("w_exp_up", [DEPTH, ne_decl, D, EH])
    C.w_exp_down = din("w_exp_down", [DEPTH, ne_decl, EH, D])
    C.w_sh_gate = din("w_sh_gate", [DEPTH, D, EH])
    C.w_sh_up = din("w_sh_up", [DEPTH, D, EH])
    C.w_sh_down = din("w_sh_down", [DEPTH, EH, D])
    C.ln2_g = din("ln2_g", [DEPTH, D])
    C.ln2_b = din("ln2_b", [DEPTH, D])
    C.cst_ident = din("cst_ident", [128, 128])
    C.cst_triu = din("cst_triu", [128, 128])
    C.cst_msb = din("cst_msb", [128, 896])
    C.cst_mfx = din("cst_mfx", [128, 896])
    C.cst_mdl = din("cst_mdl", [128, 2432])
    C.cst_invf = din("cst_invf", [128, 32])
    C.out = nc.dram_tensor("out", [SEQ, D], F32, kind="ExternalOutput").ap()
    C.xres = dscr("xres", [SEQ, D])
    C.modb = dscr("modb", [128, 6 * D])
    C.qkT = dscr("qkT", [32, 128, SEQ], BF16)
    C.vS = dscr("vS", [SEQ, D], BF16)
    C.cneg = dscr("cneg", [12, SEQ])

    S = Sched(nc)
    with ExitStack() as es:
        C.ident_f = _alloc(es, nc, 'ident_f', [128, 128], F32)
        C.ident_b = _alloc(es, nc, 'ident_b', [128, 128], BF16)
        C.ones_f = _alloc(es, nc, 'ones_f', [128, 128], F32)
        C.ones_b = _alloc(es, nc, 'ones_b', [128, 128], BF16)
        C.one1 = _alloc(es, nc, 'one1', [128, 1], F32)
        C.eps5 = _alloc(es, nc, 'eps5', [128, 1], F32)
        C.eps6 = _alloc(es, nc, 'eps6', [128, 1], F32)
        dma(S, 'sp', C.ident_f[:, :], C.cst_ident[:, :], [], ['ident_f'], 'idf')
        dma(S, 'pool', C.ident_b[:, :], C.cst_ident[:, :], [], ['ident_b'], 'idb')
        S.op('dve', lambda e: e.memset(C.ones_f[:, :], 1.0), [], ['ones_f'])
        S.op('dve', lambda e: e.memset(C.ones_b[:, :], 1.0), [], ['ones_b'])
        S.op('dve', lambda e: e.memset(C.one1[:, :], 1.0), [], ['one1'])
        S.op('dve', lambda e: e.memset(C.eps5[:, :], 1e-5), [], ['eps5'])
        S.op('dve', lambda e: e.memset(C.eps6[:, :], 1e-6), [], ['eps6'])
        S.barrier()
        for l in range(n_layers):
            xsrc = C.x if l == 0 else C.xres
            last = l == n_layers - 1
            phase_adaln(C, S, l)
            with ExitStack() as eh:
                hT = _alloc(eh, nc, 'hT', [128, 16, SEQ], BF16)
                phase_m1(C, S, l, xsrc, hT)
                phase_m2(C, S, l, hT)
            with ExitStack() as eo:
                OT = _alloc(eo, nc, 'OT', [128, 16, SEQ], BF16)
                phase_m3(C, S, l, OT)
                if stop == 'm4':
                    phase_m4(C, S, l, xsrc, C.out, OT)
                    break
                phase_m4(C, S, l, xsrc, C.xres, OT)
            phase_ffn(C, S, l, C.xres, C.out if last else C.xres)
        S.finish('sp')
    return nc


def _consts():
    p = np.arange(128)[:, None]
    u = np.arange(2432)[None, :]
    dist = u - 384 - p
    mdl = ((dist >= 0) & (dist <= 128)).astype(np.float32) \
        + ((dist >= 0) & (dist <= 512) & (dist % 4 == 0)).astype(np.float32) \
        + ((dist >= 0) & (dist <= 2048) & (dist % 16 == 0)).astype(np.float32)
    d9 = dist[:, :896]
    invf = (10000.0 ** (-np.arange(32, dtype=np.float32) / 32)).astype(np.float32)
    return {
        "cst_ident": np.eye(128, dtype=np.float32),
        "cst_triu": (p > np.arange(128)[None, :]).astype(np.float32),
        "cst_msb": (d9 > 0).astype(np.float32),
        "cst_mfx": (d9 >= 0).astype(np.float32),
        "cst_mdl": mdl.astype(np.float32),
        "cst_invf": np.ascontiguousarray(np.broadcast_to(invf[None, :], (128, 32))).astype(np.float32),
    }


def make_in_maps(inputs, cores):
    f = lambda a: np.ascontiguousarray(np.asarray(a))
    shared = {k: f(inputs[k]) for k in ("w_ada", "b_ada", "w_in", "b_forget", "w_out", "ln1_g", "ln1_b", "w_router",
                                        "router_bias", "w_exp_gate", "w_exp_up", "w_exp_down", "w_sh_gate", "w_sh_up",
                                        "w_sh_down", "ln2_g", "ln2_b")}
    shared["head_norm_g"] = f(inputs["head_norm_g"]).reshape(DEPTH, 16, 128)
    shared.update(_consts())
    x = f(inputs["x"])
    c = f(inputs["c"])
    pos = f(inputs["positions"]).astype(np.int32)
    maps = []
    for b in cores:
        m = dict(shared)
        m["x"] = x[b]
        m["c"] = c[b].reshape(16, 128)
        m["pos"] = pos[b].reshape(16, 128)
        maps.append(m)
    return maps


def kernel(**inputs):
    nc = build_program()
    in_maps = make_in_maps(inputs, list(range(N_CORES)))
    res = run_bass_kernel_spmd(nc, in_maps, core_ids=list(range(N_CORES)))
    return np.stack([np.asarray(r["out"]) for r in res.results], axis=0).astype(np.float32)
```
